# Optimizing a Trainium2 kernel written in Bass

```python
import jax, jax.numpy as jnp
from jax import lax
import numpy as np

D_MODEL = 1024
BATCH = 16
SEQ = 4096
DEPTH = 1

CHUNK = 64
CONV_CH = D_MODEL
CONV_WIDTH = 31
SG_WIDTH = D_MODEL
SG_HEADS = 8
SG_HEAD_DIM = SG_WIDTH // SG_HEADS
SG_BLOCK = 128
N_GROUPS = 4
EXPERTS_PER_GROUP = 8
N_EXPERTS = N_GROUPS * EXPERTS_PER_GROUP
TOP_K = 2
D_EXPERT = D_MODEL // 2
EXPERT_BLOCK = 256
IN_COLS = 2 * CONV_CH + 2 * SG_WIDTH + 2 * D_MODEL
EPS = 1e-6

kernel_name = "hybrid_conformer_gmlp_hmoe_block"


def rms_norm(x, g):
    xf = x.astype(jnp.float32)
    y = xf * lax.rsqrt(jnp.mean(xf * xf, axis=-1, keepdims=True) + EPS)
    return (y * g.astype(jnp.float32)).astype(x.dtype)


def layer_norm(x, g, b):
    xf = x.astype(jnp.float32)
    mu = jnp.mean(xf, axis=-1, keepdims=True)
    var = jnp.mean(jnp.square(xf - mu), axis=-1, keepdims=True)
    y = (xf - mu) * lax.rsqrt(var + EPS)
    return (y * g.astype(jnp.float32) + b.astype(jnp.float32)).astype(x.dtype)


def conformer_conv_branch(a, conv_w, conv_b, ln_g, ln_b, w_o, b_o):
    a1, a2 = jnp.split(a, 2, axis=-1)
    a = a1 * jax.nn.sigmoid(a2)
    kern = conv_w[:, None, :]
    y = lax.conv_general_dilated(
        a, kern, window_strides=(1,), padding=[(CONV_WIDTH - 1, 0)],
        dimension_numbers=("NWC", "WIO", "NWC"), feature_group_count=CONV_CH) + conv_b
    y = jax.nn.silu(layer_norm(y, ln_g, ln_b))
    return y @ w_o + b_o


def spatial_gating_branch(z, ln_g, ln_b, w_s, b_s, w_o, b_o):
    z = jax.nn.gelu(z, approximate=False)
    u, v = jnp.split(z, 2, axis=-1)
    v = layer_norm(v, ln_g, ln_b)
    bsz, seq, _ = v.shape
    n_blk = seq // SG_BLOCK
    v = v.reshape(bsz, n_blk, SG_BLOCK, SG_HEADS, SG_HEAD_DIM)
    blk = np.arange(SG_BLOCK) // CHUNK
    mask = jnp.asarray(blk[:, None] >= blk[None, :])
    w = w_s * mask[None].astype(w_s.dtype)
    sv = jnp.einsum("hqk,bnkhc->bnqhc", w, v) + b_s.T[:, :, None]
    s = u * sv.reshape(bsz, seq, SG_WIDTH)
    return s @ w_o + b_o


def hierarchical_moe(h, w_rg, b_rg, w_re, b_re, w_gate, w_up, w_down):
    bsz, seq, d = h.shape
    n_tok = bsz * seq
    xt = h.reshape(n_tok, d)
    g_logits = (xt @ w_rg + b_rg).astype(jnp.float32)
    g_prob = jax.nn.softmax(g_logits, axis=-1)
    g_sel = jnp.argmax(g_logits, axis=-1)
    g_w = jnp.take_along_axis(g_prob, g_sel[:, None], axis=-1)
    e_logits = (xt @ w_re + b_re).astype(jnp.float32).reshape(n_tok, N_GROUPS, EXPERTS_PER_GROUP)
    e_logits = jnp.take_along_axis(e_logits, g_sel[:, None, None], axis=1)[:, 0]
    top_v, top_i = lax.top_k(e_logits, TOP_K)
    e_w = jax.nn.softmax(top_v, axis=-1) * g_w
    expert_id = g_sel[:, None] * EXPERTS_PER_GROUP + top_i
    n_assign = n_tok * TOP_K
    flat_e = expert_id.reshape(-1)
    order = jnp.argsort(flat_e)
    sorted_e = flat_e[order]
    sorted_tok = order // TOP_K
    sorted_w = e_w.reshape(-1)[order]
    counts = jnp.bincount(flat_e, length=N_EXPERTS)
    starts = jnp.cumsum(counts) - counts
    padded = ((counts + EXPERT_BLOCK - 1) // EXPERT_BLOCK) * EXPERT_BLOCK
    pad_ends = jnp.cumsum(padded)
    pad_starts = pad_ends - padded
    dest = pad_starts[sorted_e] + jnp.arange(n_assign) - starts[sorted_e]
    n_blocks = -(-n_assign // EXPERT_BLOCK) + N_EXPERTS
    cap = n_blocks * EXPERT_BLOCK
    x_pad = jnp.zeros((cap, d), h.dtype).at[dest].set(xt[sorted_tok])
    block_e = jnp.minimum(
        jnp.searchsorted(pad_ends, jnp.arange(n_blocks) * EXPERT_BLOCK, side="right"), N_EXPERTS - 1)

    def run_block(args):
        xb, e = args
        return (jax.nn.silu(xb @ w_gate[e]) * (xb @ w_up[e])) @ w_down[e]

    y_pad = lax.map(run_block, (x_pad.reshape(n_blocks, EXPERT_BLOCK, d), block_e)).reshape(cap, d)
    y = y_pad[dest] * sorted_w[:, None].astype(h.dtype)
    out = jnp.zeros((n_tok, d), h.dtype).at[sorted_tok].add(y)
    return out.reshape(bsz, seq, d)


def setup_inputs(seed: int = 0) -> dict:
    key = jax.random.key(seed)
    ks = jax.random.split(key, 28)
    L, D = DEPTH, D_MODEL
    nrm = lambda k, shape, s: jax.random.normal(k, shape, jnp.float32) * s
    return {
        "x": nrm(ks[0], (BATCH, SEQ, D), 1.0),
        "norm_mix_g": 1.0 + nrm(ks[1], (L, D), 0.02),
        "w_in": nrm(ks[2], (L, D, IN_COLS), D ** -0.5),
        "b_in": nrm(ks[3], (L, IN_COLS), 0.02),
        "conv_w": nrm(ks[4], (L, CONV_WIDTH, CONV_CH), CONV_WIDTH ** -0.5),
        "conv_b": nrm(ks[5], (L, CONV_CH), 0.02),
        "conv_ln_g": 1.0 + nrm(ks[6], (L, CONV_CH), 0.02),
        "conv_ln_b": nrm(ks[7], (L, CONV_CH), 0.02),
        "w_conv_out": nrm(ks[8], (L, CONV_CH, D), CONV_CH ** -0.5),
        "b_conv_out": nrm(ks[9], (L, D), 0.02),
        "sg_ln_g": 1.0 + nrm(ks[10], (L, SG_WIDTH), 0.02),
        "sg_ln_b": nrm(ks[11], (L, SG_WIDTH), 0.02),
        "w_sg": nrm(ks[12], (L, SG_HEADS, SG_BLOCK, SG_BLOCK), SG_BLOCK ** -0.5),
        "b_sg": 1.0 + nrm(ks[13], (L, SG_HEADS, SG_BLOCK), 0.02),
        "w_sg_out": nrm(ks[14], (L, SG_WIDTH, D), SG_WIDTH ** -0.5),
        "b_sg_out": nrm(ks[15], (L, D), 0.02),
        "w_out": nrm(ks[16], (L, D, D), D ** -0.5),
        "b_out": nrm(ks[17], (L, D), 0.02),
        "norm_ffn_g": 1.0 + nrm(ks[18], (L, D), 0.02),
        "w_router_group": nrm(ks[19], (L, D, N_GROUPS), D ** -0.5),
        "b_router_group": nrm(ks[20], (L, N_GROUPS), 0.01),
        "w_router_expert": nrm(ks[21], (L, D, N_EXPERTS), D ** -0.5),
        "b_router_expert": nrm(ks[22], (L, N_EXPERTS), 0.01),
        "w_expert_gate": nrm(ks[23], (L, N_EXPERTS, D, D_EXPERT), D ** -0.5),
        "w_expert_up": nrm(ks[24], (L, N_EXPERTS, D, D_EXPERT), D ** -0.5),
        "w_expert_down": nrm(ks[25], (L, N_EXPERTS, D_EXPERT, D), D_EXPERT ** -0.5),
        "norm_final_g": 1.0 + nrm(ks[26], (D,), 0.02),
    }


def reference(x, norm_mix_g, w_in, b_in, conv_w, conv_b, conv_ln_g, conv_ln_b, w_conv_out, b_conv_out,
              sg_ln_g, sg_ln_b, w_sg, b_sg, w_sg_out, b_sg_out, w_out, b_out, norm_ffn_g,
              w_router_group, b_router_group, w_router_expert, b_router_expert,
              w_expert_gate, w_expert_up, w_expert_down, norm_final_g):
    for l in range(DEPTH):
        h = rms_norm(x, norm_mix_g[l])
        proj = h @ w_in[l] + b_in[l]
        a, z, gate_logits = jnp.split(proj, [2 * CONV_CH, 2 * CONV_CH + 2 * SG_WIDTH], axis=-1)
        y_a = conformer_conv_branch(a, conv_w[l], conv_b[l], conv_ln_g[l], conv_ln_b[l],
                                    w_conv_out[l], b_conv_out[l])
        y_b = spatial_gating_branch(z, sg_ln_g[l], sg_ln_b[l], w_sg[l], b_sg[l],
                                    w_sg_out[l], b_sg_out[l])
        g_a, g_b = jnp.split(jax.nn.sigmoid(gate_logits), 2, axis=-1)
        x = x + (g_a * y_a + g_b * y_b) @ w_out[l] + b_out[l]
        h = rms_norm(x, norm_ffn_g[l])
        x = x + hierarchical_moe(h, w_router_group[l], b_router_group[l], w_router_expert[l],
                                 b_router_expert[l], w_expert_gate[l], w_expert_up[l], w_expert_down[l])
    return rms_norm(x, norm_final_g)
```

```python
import numpy as np
import concourse.bass as bass
import concourse.mybir as mybir
from concourse.bass_utils import run_bass_kernel_spmd

F32 = mybir.dt.float32
BF16 = mybir.dt.bfloat16
I32 = mybir.dt.int32
U32 = mybir.dt.uint32
ALU = mybir.AluOpType
AF = mybir.ActivationFunctionType
AX = mybir.AxisListType


class Buf:
    __slots__ = ("name", "w", "r")

    def __init__(self, name=""):
        self.name = name
        self.w = {}
        self.r = {}


class Rec:
    ENGS = ("pe", "act", "dve", "pool", "sp")

    def __init__(self, nc):
        self.nc = nc
        self.prog = {e: [] for e in self.ENGS}
        self.sem = {}
        self.cnt = {e: 0 for e in self.ENGS}
        self.seen = {e: {} for e in self.ENGS}
        self.dsem = {}
        self._ctx = []
        for e in self.ENGS:
            cm = nc.semaphore("s_" + e)
            self.sem[e] = cm.__enter__()
            self._ctx.append(cm)

    def dma_sem(self, key):
        if key not in self.dsem:
            cm = self.nc.semaphore("d_%s" % (key,))
            s = cm.__enter__()
            self._ctx.append(cm)
            self.dsem[key] = [s, 0]
        return self.dsem[key]

    def _deps(self, eng, reads, writes, pwrites):
        need = {}
        def add(d):
            for k, (s, v) in d.items():
                if k not in need or need[k][1] < v:
                    need[k] = (s, v)
        for b in reads:
            add(b.w)
        for b in writes:
            add(b.w); add(b.r)
        for b in pwrites:
            add(b.w); add(b.r)
        waits = []
        seen = self.seen[eng]
        for k, (s, v) in need.items():
            if seen.get(k, 0) < v:
                seen[k] = v
                waits.append((s, v))
        return waits

    def _publish(self, ev, reads, writes, pwrites):
        k = id(ev[0])
        for b in reads:
            b.r[k] = ev
        for b in writes:
            b.w = {k: ev}; b.r = {}
        for b in pwrites:
            b.w[k] = ev

    def op(self, eng, fn, reads=(), writes=(), pwrites=(), inc=True):
        waits = self._deps(eng, reads, writes, pwrites)
        if inc:
            self.cnt[eng] += 1
            ev = (self.sem[eng], self.cnt[eng])
            self._publish(ev, reads, writes, pwrites)
        self.prog[eng].append((waits, fn, (self.sem[eng], 1) if inc else None))

    def group(self, eng, fns, reads=(), writes=(), pwrites=(), per_reads=None):
        n = len(fns)
        if per_reads is None:
            wl = [self._deps(eng, reads, writes, pwrites)] + [[] for _ in range(n - 1)]
            allreads = list(reads)
        else:
            wl = []
            allreads = []
            for i in range(n):
                wl.append(self._deps(eng, per_reads[i], writes if i == 0 else (), pwrites if i == 0 else ()))
                for b in per_reads[i]:
                    if b not in allreads:
                        allreads.append(b)
        self.cnt[eng] += 1
        ev = (self.sem[eng], self.cnt[eng])
        self._publish(ev, allreads, writes, pwrites)
        for i, fn in enumerate(fns):
            self.prog[eng].append((wl[i], fn, (self.sem[eng], 1) if i == n - 1 else None))

    def dma(self, eng, key, fn, reads=(), writes=(), pwrites=()):
        waits = self._deps(eng, reads, writes, pwrites)
        d = self.dma_sem(key)
        d[1] += 16
        ev = (d[0], d[1])
        self._publish(ev, reads, writes, pwrites)
        self.prog[eng].append((waits, fn, (d[0], 16)))
        return ev

    def wait_all(self, eng, bufs):
        waits = self._deps(eng, bufs, (), ())
        self.prog[eng].append((waits, None, None))

    def emit(self):
        nc = self.nc
        handles = {"pe": "tensor", "act": "scalar", "dve": "vector", "pool": "gpsimd", "sp": "sync"}
        with nc.Block() as block:
            for e in self.ENGS:
                prog = self.prog[e]

                def body(h, prog=prog):
                    for waits, fn, inc in prog:
                        for (s, v) in waits:
                            h.wait_ge(s, v)
                        if fn is None:
                            continue
                        ins = fn(h)
                        if inc is not None:
                            ins.then_inc(inc[0], inc[1])
                getattr(block, handles[e])(body)

    def close(self):
        for cm in reversed(self._ctx):
            cm.__exit__(None, None, None)


D = 1024
KC = 8
TT = 256
NSB = TT // 128
NE = 32
EPS = 1e-6
COLV = ["g1", "b_a2", "conv_b", "ln_g", "ln_b", "b_u", "b_ga", "b_gb", "sg_b", "g2"]
CI = {n: 8 * i for i, n in enumerate(COLV)}
NCOL = 8 * len(COLV)
BV = {"sg_g": 0, "b_out": 1, "g_f": 2, "b_sg": 3, "b_a1": 4, "b_co": 5, "b_sgo": 6, "b_v": 7}
BI = {"b_a1": 0, "b_co": 1, "b_sgo": 0, "b_v": 1}


class Alloc:
    def __init__(self, nc):
        self.nc = nc
        self.stack = []

    def sb(self, name, shape, dt):
        cm = self.nc.sbuf_tensor("sb_" + name, shape, dt)
        t = cm.__enter__()
        self.stack.append(cm)
        return t

    def ps(self, name, shape, dt):
        cm = self.nc.psum_tensor("ps_" + name, shape, dt)
        t = cm.__enter__()
        self.stack.append(cm)
        return t

    def free(self):
        for cm in reversed(self.stack):
            cm.__exit__(None, None, None)
        self.stack = []


def build_nc(NSEQ=2, S=4096, NB=8, debug=False):
    T = NSEQ * S
    NT = T // TT
    TPS = S // TT
    NGSB = T // 128
    CAP = NB * 128
    NROWS = NE * CAP
    nc = bass.Bass("TRN2", target_bir_lowering=False)
    dt_in = lambda name, shape: nc.dram_tensor(name, shape, F32, kind="ExternalInput").ap()
    x = dt_in("x", [T, D])
    w_in = dt_in("w_in", [D, 6 * D])
    w_co = dt_in("w_co", [D, D])
    w_sgo = dt_in("w_sgo", [D, D])
    w_out = dt_in("w_out", [D, D])
    w_gate = dt_in("w_gate", [NE, D, 512])
    w_up = dt_in("w_up", [NE, D, 512])
    w_down = dt_in("w_down", [NE, 512, D])
    cols_d = dt_in("cols", [128, NCOL])
    convw_d = dt_in("convwT", [128, KC * 31])
    wsg_d = dt_in("wsgT", [128, 8 * 128])
    mask_d = dt_in("maskT", [128, 128])
    rb_d = dt_in("rb", [1, 36])
    bvecs_d = dt_in("bvecs", [8, D])
    wr_d = dt_in("wr", [D, 36])
    out = nc.dram_tensor("out", [T, D], F32, kind="ExternalOutput").ap()
    skind = "ExternalOutput" if debug else "Internal"
    yaT = nc.dram_tensor("yaT", [D, T], BF16, kind=skind).ap()
    x1s = nc.dram_tensor("x1s", [T, D], F32, kind=skind).ap()
    xpad = nc.dram_tensor("xpad", [NROWS, D], BF16, kind=skind).ap()
    ypad = nc.dram_tensor("ypad", [NROWS, D], F32, kind=skind).ap()
    if debug:
        dbg_dest = nc.dram_tensor("dbg_dest", [128, NGSB * 2], I32, kind="ExternalOutput").ap()
        dbg_ew = nc.dram_tensor("dbg_ew", [128, NGSB * 2], F32, kind="ExternalOutput").ap()

    R = Rec(nc)
    _rr = [0]

    def alt(*engs):
        _rr[0] += 1
        return engs[_rr[0] % len(engs)]

    def barrier():
        evs = {}
        for e in R.ENGS:
            if R.cnt[e] > 0:
                evs[id(R.sem[e])] = (R.sem[e], R.cnt[e])
        for k, (s, c) in R.dsem.items():
            if c > 0:
                evs[id(s)] = (s, c)
        for e in R.ENGS:
            waits = []
            for k, (s, v) in evs.items():
                if R.seen[e].get(k, 0) < v:
                    R.seen[e][k] = v
                    waits.append((s, v))
            R.prog[e].append((waits, None, None))

    PA = Alloc(nc)
    cols = PA.sb("cols", [128, NCOL], F32)
    colsh = PA.sb("colsh", [128, NCOL], F32)
    identf = PA.sb("identf", [128, 128], F32)
    identb = PA.sb("identb", [128, 128], BF16)
    ones_t = PA.sb("ones_t", [128, 512], BF16)
    onesf = PA.sb("onesf", [128, 128], F32)
    rbbc = PA.sb("rbbc", [128, 36], F32)
    negh = PA.sb("negh", [128, 256], F32)
    junk = [PA.sb("junk%d" % i, [128, D], BF16) for i in range(1)] * 2
    b_junk = [Buf("junk0")] * 2
    _jk = [0]
    iot = PA.sb("iot", [128, 128], I32)
    iotf = PA.sb("iotf", [128, 128], F32)
    DEST = PA.sb("DEST", [128, NGSB, 2], I32)
    GLM = PA.sb("GLM", [128, NGSB, 4], F32)
    DV = PA.sb("DV", [128, NGSB], F32)
    EW0 = PA.sb("EW0", [128, NGSB], F32)
    EW1 = PA.sb("EW1", [128, NGSB], F32)
    b_cols, b_colsh, b_identf, b_identb, b_ones, b_rows, b_negh, b_iot, b_Bbc = (Buf(n) for n in
        ["cols", "colsh", "identf", "identb", "ones", "rows", "negh", "iot", "Bbc"])
    b_DEST = [Buf("DEST%d" % i) for i in range(NGSB)]
    b_GLM = Buf("GLM"); b_DV = Buf("DV"); b_EW = Buf("EW")

    R.dma("sp", "c0", lambda h: h.dma_start(out=cols[:], in_=cols_d[:, :]), writes=[b_cols])
    R.dma("sp", "c1", lambda h: h.dma_start(out=rbbc[:], in_=rb_d[0:1, :].partition_broadcast(128)), writes=[b_rows])
    R.op("dve", lambda h: h.tensor_scalar(out=rbbc[:], in0=rbbc[:], scalar1=1.0 / 128, scalar2=None, op0=ALU.mult),
         reads=[b_rows], writes=[b_rows])
    R.op("pool", lambda h: h.iota(iot[:], pattern=[[1, 128]], base=0, channel_multiplier=-1), writes=[b_iot])
    R.op("dve", lambda h: h.tensor_copy(out=iotf[:], in_=iot[:]), reads=[b_iot], writes=[b_iot])
    R.op("dve", lambda h: h.tensor_scalar(out=identf[:], in0=iotf[:], scalar1=0.0, scalar2=None, op0=ALU.is_equal),
         reads=[b_iot], writes=[b_identf])
    R.op("dve", lambda h: h.tensor_copy(out=identb[:], in_=identf[:]), reads=[b_identf], writes=[b_identb])
    R.op("pool", lambda h: h.memset(ones_t[:], 1.0), writes=[b_ones])
    R.op("pool", lambda h: h.memset(onesf[:], 1.0), pwrites=[b_ones])
    R.op("pool", lambda h: h.memset(negh[:], -0.5), writes=[b_negh])
    R.op("dve", lambda h: h.tensor_scalar(out=colsh[:], in0=cols[:], scalar1=0.5, scalar2=None, op0=ALU.mult),
         reads=[b_cols], writes=[b_colsh])

    def col(name, j):
        return cols[:, CI[name] + j:CI[name] + j + 1]

    def colh(name, j):
        return colsh[:, CI[name] + j:CI[name] + j + 1]

    def mm_group(psum_ap, psum_buf, terms, partial=None):
        n = len(terms)
        first, last = (True, True) if partial is None else partial
        fns = []
        per = []
        for i, (l, r, bs) in enumerate(terms):
            fns.append(lambda h, l=l, r=r, st=(first and i == 0), sp=(last and i == n - 1):
                       h.matmul(psum_ap, lhsT=l, rhs=r, start=st, stop=sp))
            per.append(list(bs))
        if first:
            R.group("pe", fns, writes=[psum_buf], per_reads=per)
        else:
            R.group("pe", fns, pwrites=[psum_buf], per_reads=per)

    def rms_head(xs_ap, b_xs, ss_ap, rs_ap, b_st):
        _jk[0] ^= 1
        jk = _jk[0]
        R.op("act", lambda h: h.activation(out=junk[jk][:], in_=xs_ap, func=AF.Square, accum_out=ss_ap),
             reads=[b_xs], writes=[b_st, b_junk[jk]])
        R.op("dve", lambda h: h.tensor_scalar(out=ss_ap, in0=ss_ap, scalar1=1.0 / D, scalar2=EPS,
                                              op0=ALU.mult, op1=ALU.add), reads=[b_st], writes=[b_st])
        R.op("pool", lambda h: h.tensor_tensor(out=rs_ap, in0=ss_ap, in1=negh[:, 0:1], op=ALU.pow),
             reads=[b_st, b_negh], writes=[b_st])

    def cast_op(eng, out_ap, in_ap, scale, reads, pwrites):
        if eng == "act":
            if scale is None:
                R.op("act", lambda h: h.activation(out=out_ap, in_=in_ap, func=AF.Copy), reads=reads, pwrites=pwrites)
            else:
                R.op("act", lambda h: h.activation(out=out_ap, in_=in_ap, func=AF.Identity, scale=scale), reads=reads, pwrites=pwrites)
        else:
            if scale is None:
                R.op("dve", lambda h: h.tensor_copy(out=out_ap, in_=in_ap), reads=reads, pwrites=pwrites)
            else:
                R.op("dve", lambda h: h.tensor_scalar(out=out_ap, in0=in_ap, scalar1=scale, scalar2=None, op0=ALU.mult),
                     reads=reads, pwrites=pwrites)

    A1 = Alloc(nc)
    Bbc = A1.sb("Bbc1", [128, 2, D], BF16)
    Wa = A1.sb("Wa", [128, KC, 3072], BF16)
    Wco = A1.sb("Wco", [128, KC, D], BF16)
    Dg = A1.sb("Dg", [128, KC, 31, 128], BF16)
    cw = A1.sb("cw", [128, KC * 31], F32)
    onesM = A1.sb("onesM", [128, 128], BF16)
    A1s = Alloc(nc)
    stage = [A1s.sb("stage%d" % i, [128, 2048], F32) for i in range(2)]
    b_Wa = [Buf("Wa%d" % k) for k in range(KC)]
    b_Wco = [Buf("Wco%d" % k) for k in range(KC)]
    b_Dg = [Buf("Dg%d" % j) for j in range(KC)]
    b_cw = Buf("cw"); b_onesM = Buf("onesM")
    b_stage = [Buf("stage0"), Buf("stage1")]
    b_xs = [Buf("xs%d" % i) for i in range(2)]
    b_hb = [Buf("hb%d" % i) for i in range(2)]
    b_st1 = [Buf("st1_%d" % i) for i in range(2)]
    b_hT = [[Buf("hT%d_%d" % (i, s)) for s in range(NSB)] for i in range(2)]
    b_Gm = [Buf("Gm%d" % j) for j in range(KC)]
    b_Gh = [Buf("Gh%d" % j) for j in range(KC)]
    b_th = [Buf("th0"), Buf("th1")]
    b_Y = [Buf("Y%d" % j) for j in range(KC)]
    b_Yb = [Buf("Yb%d" % j) for j in range(KC)]
    b_Ysq = [Buf("Ysq0"), Buf("Ysq1")]
    b_lnr = Buf("lnr"); b_lnv = Buf("lnv"); b_Dr = Buf("Dr")
    b_dn = [Buf("dn0"), Buf("dn1")]
    b_dn2 = [Buf("dn2_0"), Buf("dn2_1")]
    b_A = [Buf("A%d" % j) for j in range(KC)]
    b_tga = [Buf("tga%d" % j) for j in range(KC)]
    b_ost = [[Buf("ost%d_%d" % (i, j)) for j in range(KC)] for i in range(1)]
    b_tr = [Buf("tr0"), Buf("tr1")]
    b_mmb = [Buf("mmb%d" % i) for i in range(4)]
    b_stM = Buf("stM"); b_stQ = Buf("stQ")
    b_yaT = [Buf("yaT%d" % t) for t in range(NT)]
    _mm = [0]

    def next_bank():
        _mm[0] = (_mm[0] + 1) % len(mmb)
        return mmb[_mm[0]], b_mmb[_mm[0]]

    _st = [0]

    def load_cast(dram_ap, ncols, out_ap, out_buf, scale_ap=None, extra_reads=()):
        _st[0] ^= 1
        sl = _st[0]
        R.dma("sp", "stg%d" % sl, lambda h: h.dma_start(out=stage[sl][:, 0:ncols], in_=dram_ap), writes=[b_stage[sl]])
        cast_op(alt("dve", "act"), out_ap, stage[sl][:, 0:ncols], scale_ap, [b_stage[sl], b_cols], [out_buf])

    R.dma("sp", "c2", lambda h: h.dma_start(out=cw[:], in_=convw_d[:, :]), writes=[b_cw])
    R.op("pool", lambda h: h.memset(onesM[:], 1.0 / D), writes=[b_onesM])
    for k in range(KC):
        rs = slice(k * 128, (k + 1) * 128)
        load_cast(w_in[rs, 0:2048], 2048, Wa[:, k, 0:2048], b_Wa[k], col("g1", k))
        load_cast(w_in[rs, 4096:5120], 1024, Wa[:, k, 2048:3072], b_Wa[k], col("g1", k))
        load_cast(w_co[rs, :], 1024, Wco[:, k, :], b_Wco[k])
    R.op("dve", lambda h: h.tensor_scalar(out=cw[:], in0=cw[:], scalar1=0.5, scalar2=None, op0=ALU.mult),
         reads=[b_cw], writes=[b_cw])
    for j in range(KC):
        for tap in range(31):
            cast_op(alt("dve", "act"), Dg[:, j, tap, :], identf[:], cw[:, j * 31 + tap:j * 31 + tap + 1],
                    [b_identf, b_cw], [b_Dg[j]])
    for i, nm in enumerate(["b_a1", "b_co"]):
        _st[0] ^= 1
        sl = _st[0]
        R.dma("sp", "stg%d" % sl, lambda h, sl=sl, nm=nm: h.dma_start(out=stage[sl][:, 0:D],
              in_=bvecs_d[BV[nm]:BV[nm] + 1, :].partition_broadcast(128)), writes=[b_stage[sl]])
        R.op("dve", lambda h, sl=sl, i=i: h.tensor_scalar(out=Bbc[:, i, :], in0=stage[sl][:, 0:D], scalar1=1.0 / 128, scalar2=None,
                                                         op0=ALU.mult), reads=[b_stage[sl]], pwrites=[b_Bbc])
    barrier()
    A1s.free()
    xs = [A1.sb("xs%d" % i, [128, D], F32) for i in range(2)]
    hb = [A1.sb("hb%d" % i, [128, D], BF16) for i in range(2)]
    st1 = [A1.sb("st1_%d" % i, [128, 2], F32) for i in range(2)]
    hT = [A1.sb("hT%d" % i, [128, KC, TT], BF16) for i in range(2)]
    G = A1.sb("G", [128, KC, 32 + TT], BF16)
    th = [A1.sb("th%d" % i, [128, TT], F32) for i in range(2)]
    Y = A1.sb("Y", [128, KC, TT], F32)
    Yb = A1.sb("Yb", [128, KC, TT], BF16)
    Ysq = [A1.sb("Ysq%d" % i, [128, TT], BF16) for i in range(2)]
    lnr = A1.sb("lnr", [128, 2, 2], F32)
    lnv = A1.sb("lnv", [128, 4, 2], F32)
    Dr = A1.sb("Dr", [128, 2, 2, 128], F32)
    dn = [A1.sb("dn%d" % i, [128, TT], F32) for i in range(2)]
    dn2 = [A1.sb("dn2_%d" % i, [128, TT], F32) for i in range(2)]
    Aact = A1.sb("Aact", [128, KC, TT], BF16)
    tga = A1.sb("tga", [128, KC, TT], BF16)
    ostage = [A1.sb("ostage%d" % i, [128, KC, TT], BF16) for i in range(1)]
    tr = [A1.ps("tr%d" % i, [128, KC, 128], BF16) for i in range(2)]
    mmb = [A1.ps("mmb%d" % i, [128, 512], F32) for i in range(4)]
    stM = A1.ps("stM", [128, 512], F32)
    stQ = A1.ps("stQ", [128, 2, 2, 128], F32)
    R.op("pool", lambda h: h.memset(G[:, :, 0:32], 0.0), writes=b_Gh)

    def p1_head_a(t):
        for sbi in range(NSB):
            xsl = sbi
            tok0 = t * TT + sbi * 128
            R.dma("sp", "xs%d" % xsl, lambda h, xsl=xsl, tok0=tok0: h.dma_start(out=xs[xsl][:], in_=x[tok0:tok0 + 128, :]),
                  writes=[b_xs[xsl]])
            rms_head(xs[xsl][:], b_xs[xsl], st1[xsl][:, 0:1], st1[xsl][:, 1:2], b_st1[xsl])
            R.op("dve", lambda h, xsl=xsl, sbi=sbi: h.tensor_scalar(out=hb[sbi][:], in0=xs[xsl][:], scalar1=st1[xsl][:, 1:2],
                                                                     scalar2=None, op0=ALU.mult),
                 reads=[b_xs[xsl], b_st1[xsl]], writes=[b_hb[sbi]])

    def p1_head_b(t):
        ts = t % 2
        for sbi in range(NSB):
            R.group("pe", [(lambda h, k=k, sbi=sbi: h.transpose(out=tr[sbi][:, k, :], in_=hb[sbi][:, k * 128:(k + 1) * 128],
                                                               identity=identb[:])) for k in range(KC)],
                    reads=[b_hb[sbi], b_identb], writes=[b_tr[sbi]])
            R.op("act", lambda h, sbi=sbi, ts=ts: h.activation(out=hT[ts][:, :, sbi * 128:(sbi + 1) * 128], in_=tr[sbi][:],
                                                               func=AF.Copy),
                 reads=[b_tr[sbi]], writes=[b_hT[ts][sbi]])

    def p1_A(t, j):
        ts = t % 2
        hTr = b_hT[ts]
        p1, bp1 = next_bank()
        terms = [(Bbc[:, BI["b_a1"], j * 128:(j + 1) * 128], ones_t[:, 0:TT], [b_Bbc, b_ones])]
        terms += [(Wa[:, k, j * 128:(j + 1) * 128], hT[ts][:, k, :], [b_Wa[k]] + hTr) for k in range(KC)]
        mm_group(p1[:, 0:TT], bp1, terms)
        p2, bp2 = next_bank()
        terms = [(Wa[:, k, D + j * 128:D + (j + 1) * 128], hT[ts][:, k, :], [b_Wa[k]] + hTr) for k in range(KC)]
        mm_group(p2[:, 0:TT], bp2, terms)
        tsl = j % 2
        R.op("act", lambda h: h.activation(out=th[tsl][:], in_=p2[:, 0:TT], func=AF.Tanh, bias=colh("b_a2", j), scale=0.5),
             reads=[bp2, b_colsh], writes=[b_th[tsl]])
        R.op("dve", lambda h: h.scalar_tensor_tensor(out=G[:, j, 32:32 + TT], in0=th[tsl][:], scalar=1.0, in1=p1[:, 0:TT],
                                                     op0=ALU.add, op1=ALU.mult),
             reads=[b_th[tsl], bp1], writes=[b_Gm[j]])

    def p1_C(t, j, last_in_seq):
        pc, bpc = next_bank()
        terms = [(Dg[:, j, tap, :], G[:, j, 2 + tap:2 + tap + TT], [b_Dg[j], b_Gm[j], b_Gh[j]]) for tap in range(31)]
        mm_group(pc[:, 0:TT], bpc, terms)
        R.op("act", lambda h: h.activation(out=Y[:, j, :], in_=pc[:, 0:TT], func=AF.Identity, bias=col("conv_b", j), scale=1.0),
             reads=[bpc, b_cols], writes=[b_Y[j]])
        if last_in_seq:
            R.op("pool", lambda h: h.memset(G[:, j, 0:32], 0.0), writes=[b_Gh[j]])
        else:
            R.op("pool", lambda h: h.tensor_copy(out=G[:, j, 0:32], in_=G[:, j, TT:TT + 32]), reads=[b_Gm[j]], writes=[b_Gh[j]])
        R.op("dve", lambda h: h.tensor_copy(out=Yb[:, j, :], in_=Y[:, j, :]), reads=[b_Y[j]], writes=[b_Yb[j]])
        qs = j % 2
        R.op("act", lambda h: h.activation(out=Ysq[qs][:], in_=Y[:, j, :], func=AF.Square), reads=[b_Y[j]], writes=[b_Ysq[qs]])

    def p1_S(t, j):
        qs = j % 2
        fns = []
        for sb in range(NSB):
            c0 = (sb * 2 + 0) * 8 + j
            c1 = (sb * 2 + 1) * 8 + j
            fns.append(lambda h, sb=sb, c0=c0: h.matmul(stM[:, c0:c0 + 1], lhsT=Yb[:, j, sb * 128:(sb + 1) * 128], rhs=onesM[:, 0:1],
                                                        start=True, stop=True))
            fns.append(lambda h, sb=sb, c1=c1: h.matmul(stM[:, c1:c1 + 1], lhsT=Ysq[qs][:, sb * 128:(sb + 1) * 128], rhs=onesM[:, 0:1],
                                                        start=True, stop=True))
        if j == 0:
            R.group("pe", fns, reads=[b_onesM, b_Yb[j], b_Ysq[qs]], writes=[b_stM])
        else:
            R.group("pe", fns, reads=[b_onesM, b_Yb[j], b_Ysq[qs]], pwrites=[b_stM])

    def p1_gates(t, jos=None):
        ts = t % 2
        for jo in (range(KC) if jos is None else jos):
            pg, bpg = next_bank()
            terms = [(Wa[:, k, 2048 + jo * 128:2048 + (jo + 1) * 128], hT[ts][:, k, :], [b_Wa[k]] + b_hT[ts]) for k in range(KC)]
            mm_group(pg[:, 0:TT], bpg, terms)
            R.op("act", lambda h, jo=jo, pg=pg: h.activation(out=tga[:, jo, :], in_=pg[:, 0:TT], func=AF.Tanh,
                                                             bias=colh("b_ga", jo), scale=0.5),
                 reads=[bpg, b_colsh], writes=[b_tga[jo]])

    def p1_tail(t):
        os_ = 0
        R.op("dve", lambda h: h.tensor_reduce(out=lnr[:], in_=stM[:, 0:32].rearrange("p (a j) -> p a j", j=8), axis=AX.X, op=ALU.add),
             reads=[b_stM], writes=[b_lnr])
        R.op("dve", lambda h: h.tensor_tensor(out=lnv[:, 0, :], in0=lnr[:, :, 0], in1=lnr[:, :, 0], op=ALU.mult),
             reads=[b_lnr], writes=[b_lnv])
        R.op("dve", lambda h: h.scalar_tensor_tensor(out=lnv[:, 1, :], in0=lnr[:, :, 1], scalar=EPS, in1=lnv[:, 0, :],
                                                     op0=ALU.add, op1=ALU.subtract), reads=[b_lnr, b_lnv], writes=[b_lnv])
        R.op("pool", lambda h: h.tensor_tensor(out=lnv[:, 2, :], in0=lnv[:, 1, :], in1=negh[:, 0:2], op=ALU.pow),
             reads=[b_lnv, b_negh], writes=[b_lnv])
        R.op("dve", lambda h: h.scalar_tensor_tensor(out=lnv[:, 3, :], in0=lnr[:, :, 0], scalar=-1.0, in1=lnv[:, 2, :],
                                                     op0=ALU.mult, op1=ALU.mult), reads=[b_lnr, b_lnv], writes=[b_lnv])
        for sb in range(NSB):
            for st_ in range(2):
                R.op("dve", lambda h, sb=sb, st_=st_: h.tensor_scalar(out=Dr[:, sb, st_, :], in0=identf[:],
                                                                      scalar1=lnv[:, 2 + st_, sb:sb + 1], scalar2=None, op0=ALU.mult),
                     reads=[b_identf, b_lnv], pwrites=[b_Dr])

    def p1_tail_mm(t):
        R.group("pe", [(lambda h, sb=sb: h.matmul(stQ[:, sb, :, :], lhsT=onesf[:], rhs=Dr[:, sb, :, :], start=True, stop=True))
                       for sb in range(NSB)], reads=[b_ones, b_Dr], writes=[b_stQ])

    def p1_tail_c(t, js=None):
        for j in (range(KC) if js is None else js):
            s2 = j % 2
            R.op("dve", lambda h, j=j, s2=s2: h.tensor_tensor(out=dn[s2][:].rearrange("p (a t) -> p a t", a=2),
                                                              in0=Y[:, j, :].rearrange("p (a t) -> p a t", a=2),
                                                              in1=stQ[:, :, 0, :], op=ALU.mult),
                 reads=[b_Y[j], b_stQ], writes=[b_dn[s2]])
            R.op("dve", lambda h, s2=s2: h.tensor_tensor(out=dn2[s2][:].rearrange("p (a t) -> p a t", a=2),
                                                         in0=dn[s2][:].rearrange("p (a t) -> p a t", a=2),
                                                         in1=stQ[:, :, 1, :], op=ALU.add),
                 reads=[b_dn[s2], b_stQ], writes=[b_dn2[s2]])
            R.op("act", lambda h, j=j, s2=s2: h.activation(out=Aact[:, j, :], in_=dn2[s2][:], func=AF.Silu,
                                                           scale=col("ln_g", j), bias=col("ln_b", j)),
                 reads=[b_dn2[s2], b_cols], writes=[b_A[j]])

    def p1_po(t):
        os_ = 0
        for jo in range(KC):
            po, bpo = next_bank()
            terms = [(Bbc[:, BI["b_co"], jo * 128:(jo + 1) * 128], ones_t[:, 0:TT], [b_Bbc, b_ones])]
            terms += [(Wco[:, k, jo * 128:(jo + 1) * 128], Aact[:, k, :], [b_Wco[k], b_A[k]]) for k in range(KC)]
            mm_group(po[:, 0:TT], bpo, terms)
            R.op("dve", lambda h, jo=jo, po=po: h.scalar_tensor_tensor(out=ostage[os_][:, jo, :], in0=tga[:, jo, :], scalar=1.0,
                                                                        in1=po[:, 0:TT], op0=ALU.add, op1=ALU.mult),
                 reads=[b_tga[jo], bpo], writes=[b_ost[os_][jo]])
        R.dma("sp", "ost%d" % os_, lambda h: h.dma_start(
            out=yaT.rearrange("(j p) t -> p j t", p=128)[:, :, t * TT:(t + 1) * TT], in_=ostage[os_][:]),
            reads=b_ost[os_], writes=[b_yaT[t]])

    def p1_body(t, i0, i1):
        last_in_seq = (t % TPS == TPS - 1)
        for i in range(i0, i1):
            if i < KC:
                p1_A(t, i)
            if 1 <= i <= KC:
                p1_C(t, i - 1, last_in_seq)
            if i >= 2:
                p1_S(t, i - 2)

    p1_head_a(0)
    p1_head_b(0)
    p1_body(0, 0, 2)
    for t in range(NT):
        nxt = t + 1 < NT
        if nxt:
            p1_head_a(t + 1)
        p1_body(t, 2, KC + 2)
        p1_tail(t)
        if nxt:
            p1_head_b(t + 1)
        p1_tail_mm(t)
        for j in range(KC):
            p1_tail_c(t, [j])
            p1_gates(t, [j])
        p1_po(t)
        if nxt:
            p1_body(t + 1, 0, 2)
    barrier()
    A1.free()

    A2 = Alloc(nc)
    Bbc2 = A2.sb("Bbc2", [128, 2, D], BF16)
    Wz = A2.sb("Wz", [128, KC, 2048], BF16)
    Wgb = A2.sb("Wgb", [128, KC, D], BF16)
    Wsgo = A2.sb("Wsgo", [128, KC, D], BF16)
    Wout = A2.sb("Wout", [128, KC, D], BF16)
    WmT = A2.sb("WmT", [128, 8, 128], BF16)
    Cst = A2.sb("Cst", [128, 8, 128], F32)
    Gbc = A2.sb("Gbc", [128, D], F32)
    Boutbc = A2.sb("Boutbc", [128, D], F32)
    Wr = A2.sb("Wr", [128, KC, 36], F32)
    iox = A2.sb("iox", [128, 32], I32)
    iotx32 = A2.sb("iotx32", [128, 32], F32)
    iotg32 = A2.sb("iotg32", [128, 32], F32)
    Ustrict = A2.sb("Ustrict", [128, 128], BF16)
    Acum = [A2.sb("Acum%d" % i, [128, 32], BF16) for i in range(2)]
    b_Wz = [Buf("Wz%d" % k) for k in range(KC)]
    b_Wgb = [Buf("Wgb%d" % k) for k in range(KC)]
    b_Wsgo = [Buf("Wsgo%d" % k) for k in range(KC)]
    b_Wout = [Buf("Wout%d" % k) for k in range(KC)]
    b_WmT = Buf("WmT"); b_Cst = Buf("Cst"); b_Gbc = Buf("Gbc"); b_Bout = Buf("Bout"); b_Wr = Buf("Wr")
    b_rc = Buf("routeconst"); b_Acum = [Buf("Acum0"), Buf("Acum1")]

    A2s = Alloc(nc)
    stageB = [A2s.sb("stage2_%d" % i, [128, 2048], F32) for i in range(2)]
    b_stage = [Buf("stage0"), Buf("stage1")]
    wsg_f = A2s.sb("wsg_f", [128, 8 * 128], F32)
    maskf = A2s.sb("maskf", [128, 128], F32)
    bsbc = A2s.sb("bsbc", [128, D], F32)
    wr_f = A2s.sb("wr_f", [128, KC, 36], F32)
    rwp = A2s.ps("rwp", [128, 512], F32)
    b_wsgf = Buf("wsgf"); b_maskf = Buf("maskf"); b_bsbc = Buf("bsbc"); b_wrf = Buf("wrf"); b_rwp = Buf("rwp")

    def load_cast2(dram_ap, ncols, out_ap, out_buf, scale=None):
        _st[0] ^= 1
        sl = _st[0]
        R.dma("sp", "stg%d" % sl, lambda h: h.dma_start(out=stageB[sl][:, 0:ncols], in_=dram_ap), writes=[b_stage[sl]])
        cast_op(alt("dve", "act"), out_ap, stageB[sl][:, 0:ncols], scale, [b_stage[sl], b_cols], [out_buf])

    for k in range(KC):
        rs = slice(k * 128, (k + 1) * 128)
        load_cast2(w_in[rs, 2048:4096], 2048, Wz[:, k, :], b_Wz[k], col("g1", k))
        load_cast2(w_in[rs, 5120:6144], 1024, Wgb[:, k, :], b_Wgb[k], col("g1", k))
        load_cast2(w_sgo[rs, :], 1024, Wsgo[:, k, :], b_Wsgo[k])
        load_cast2(w_out[rs, :], 1024, Wout[:, k, :], b_Wout[k], 0.5)
    for i, nm in enumerate(["b_sgo", "b_v"]):
        _st[0] ^= 1
        sl = _st[0]
        R.dma("sp", "stg%d" % sl, lambda h, sl=sl, nm=nm: h.dma_start(out=stageB[sl][:, 0:D],
              in_=bvecs_d[BV[nm]:BV[nm] + 1, :].partition_broadcast(128)), writes=[b_stage[sl]])
        R.op("dve", lambda h, sl=sl, i=i: h.tensor_scalar(out=Bbc2[:, i, :], in0=stageB[sl][:, 0:D], scalar1=1.0 / 128, scalar2=None,
                                                         op0=ALU.mult), reads=[b_stage[sl]], pwrites=[b_Bbc])
    R.dma("sp", "c3", lambda h: h.dma_start(out=wsg_f[:], in_=wsg_d[:, :]), writes=[b_wsgf])
    R.dma("sp", "c4", lambda h: h.dma_start(out=maskf[:], in_=mask_d[:, :]), writes=[b_maskf])
    R.dma("sp", "c5", lambda h: h.dma_start(out=Gbc[:], in_=bvecs_d[BV["sg_g"]:BV["sg_g"] + 1, :].partition_broadcast(128)), writes=[b_Gbc])
    R.dma("sp", "c6", lambda h: h.dma_start(out=Boutbc[:], in_=bvecs_d[BV["b_out"]:BV["b_out"] + 1, :].partition_broadcast(128)), writes=[b_Bout])
    R.dma("sp", "c7", lambda h: h.dma_start(out=bsbc[:], in_=bvecs_d[BV["b_sg"]:BV["b_sg"] + 1, :].partition_broadcast(128)), writes=[b_bsbc])
    R.dma("sp", "c8", lambda h: h.dma_start(out=wr_f[:], in_=wr_d.rearrange("(k p) n -> p k n", p=128)), writes=[b_wrf])
    R.op("dve", lambda h: h.tensor_scalar(out=Ustrict[:], in0=iotf[:], scalar1=0.0, scalar2=None, op0=ALU.is_gt),
         reads=[b_iot], writes=[b_rc])
    R.op("pool", lambda h: h.iota(iox[:], pattern=[[1, 32]], base=0, channel_multiplier=0), pwrites=[b_rc])
    R.op("dve", lambda h: h.tensor_copy(out=iotx32[:], in_=iox[:]), reads=[b_rc], pwrites=[b_rc])
    R.op("pool", lambda h: h.iota(iox[:], pattern=[[1, 4], [0, 8]], base=0, channel_multiplier=0), reads=[b_rc], pwrites=[b_rc])
    R.op("dve", lambda h: h.tensor_copy(out=iotg32[:], in_=iox[:]), reads=[b_rc], pwrites=[b_rc])
    R.op("pool", lambda h: h.memset(Acum[0][:], 0.0), writes=[b_Acum[0]])
    for k in range(KC):
        R.op("dve", lambda h, k=k: h.tensor_scalar(out=Wr[:, k, :], in0=wr_f[:, k, :], scalar1=col("g2", k), scalar2=None,
                                                   op0=ALU.mult), reads=[b_wrf, b_cols], pwrites=[b_Wr])
    for hd in range(8):
        R.op("dve", lambda h, hd=hd: h.tensor_tensor(out=WmT[:, hd, :], in0=wsg_f[:, hd * 128:(hd + 1) * 128], in1=maskf[:],
                                                     op=ALU.mult), reads=[b_wsgf, b_maskf], pwrites=[b_WmT])
    for hd in range(8):
        R.group("pe", [lambda h, hd=hd: h.matmul(rwp[:, 0:128], lhsT=ones_t[:, 0:128], rhs=WmT[:, hd, :], start=True, stop=True)],
                reads=[b_ones, b_WmT], writes=[b_rwp])
        R.op("dve", lambda h, hd=hd: h.scalar_tensor_tensor(out=Cst[:, hd, :], in0=rwp[:, 0:128], scalar=col("sg_b", hd),
                                                            in1=bsbc[:, hd * 128:(hd + 1) * 128], op0=ALU.mult, op1=ALU.add),
             reads=[b_rwp, b_cols, b_bsbc], pwrites=[b_Cst])
    barrier()
    A2s.free()

    xs2 = [A2.sb("xs2_%d" % i, [128, D], F32) for i in range(6)]
    hbB = [A2.sb("hb2_%d" % i, [128, D], BF16) for i in range(2)]
    st2 = [A2.sb("st2_%d" % i, [128, 4], F32) for i in range(6)]
    hTB = [A2.sb("hT2_%d" % i, [128, KC, TT], BF16) for i in range(1)] * 2
    uT = A2.sb("uT", [128, KC, TT], BF16)
    v = [A2.sb("v%d" % i, [128, D], F32) for i in range(2)]
    bst = [A2.sb("bst%d" % i, [128, 2, 6], F32) for i in range(2)]
    mv = [A2.sb("mv%d" % i, [128, 4], F32) for i in range(2)]
    vn = [A2.sb("vn%d" % i, [128, D], BF16) for i in range(2)]
    tmpS = [A2.sb("tmpS%d" % i, [128, 8, 128], F32) for i in range(1)] * 2
    sT = A2.sb("sT", [128, KC, TT], BF16)
    tgb = A2.sb("tgb", [128, KC, TT], BF16)
    gaya = [A2.sb("gaya%d" % i, [128, KC, TT], BF16) for i in range(1)] * 2
    t1 = [A2.sb("t1_%d" % i, [128, TT], F32) for i in range(2)]
    mixT = A2.sb("mixT", [128, KC, TT], BF16)
    h2f = [A2.sb("h2f%d" % i, [128, D], F32) for i in range(2)]
    h2b = [A2.sb("h2b%d" % i, [128, D], BF16) for i in range(4)]
    h2T = [A2.sb("h2T%d" % i, [128, KC, 128], F32) for i in range(1)] * 2
    rt = []
    for i in range(4):
        d = {}
        for nm, shp, dty in [("L", [128, 36], F32), ("gl8", [128, 8], F32), ("t8g", [128, 8], F32), ("i8g", [128, 8], U32),
                             ("gself", [128, 1], F32), ("pen32", [128, 32], F32), ("M32", [128, 32], F32),
                             ("t8e", [128, 8], F32), ("i8e", [128, 8], U32), ("ef", [128, 2], F32),
                             ("A0", [128, 32], F32), ("A1", [128, 32], F32), ("Ab", [128, 32], BF16),
                             ("prod", [128, 32], F32), ("pos", [128, 2], F32), ("dest", [128, 2], F32)]:
            d[nm] = A2.sb("rt%d_%s" % (i, nm), shp, dty)
            d["b_" + nm] = Buf("rt%d_%s" % (i, nm))
        rt.append(d)
    tr2 = A2.ps("tr2", [128, KC, 128], BF16)
    big = [A2.ps("big%d" % i, [128, KC, 128], F32) for i in range(2)]
    mmbB = [A2.ps("mmb2_%d" % i, [128, 512], F32) for i in range(3)]
    b_xs2 = [Buf("xs2_%d" % i) for i in range(6)]
    b_hb = [Buf("hb0"), Buf("hb1")]
    b_st2 = [Buf("st2_%d" % i) for i in range(6)]
    b_hT = [[Buf("hT2_%d_%d" % (i, s)) for s in range(NSB)] for i in range(1)] * 2
    b_uT = [Buf("uT%d" % j) for j in range(KC)]
    b_v = [[Buf("v%d_%d" % (i, n)) for n in range(2)] for i in range(2)]
    b_bst = [Buf("bst0"), Buf("bst1")]
    b_mv = [Buf("mv0"), Buf("mv1")]
    b_vn = [Buf("vn0"), Buf("vn1")]
    b_tmpS = [Buf("tmpS0")] * 2
    b_sT = [Buf("sT%d" % s) for s in range(NSB)]
    b_tgb = [Buf("tgb%d" % j) for j in range(KC)]
    b_gaya = [Buf("gaya0")] * 2
    b_t1 = [Buf("t1_0"), Buf("t1_1")]
    b_mixT = [Buf("mixT%d" % j) for j in range(KC)]
    b_h2f = [Buf("h2f0"), Buf("h2f1")]
    b_h2b = [Buf("h2b%d" % i) for i in range(4)]
    b_h2T = [Buf("h2T0")] * 2
    b_tr2 = Buf("tr2"); b_big = [Buf("big0"), Buf("big1")]
    b_mmb = [Buf("mmb2_%d" % i) for i in range(3)]
    b_x1s = [Buf("x1s%d" % i) for i in range(NGSB)]
    b_xpad = Buf("xpad")
    _mm[0] = 0
    _bg = [0]

    def next_bank2():
        _mm[0] = (_mm[0] + 1) % len(mmbB)
        return mmbB[_mm[0]], b_mmb[_mm[0]]

    def next_big():
        _bg[0] ^= 1
        return big[_bg[0]], b_big[_bg[0]]

    for i in range(4):
        R.op("pool", lambda h, i=i: h.memset(rt[i]["gl8"][:], -1e30), writes=[rt[i]["b_gl8"]])

    def p2_gaya(t):
        ts = t % 2
        R.dma("sp", "gaya%d" % ts, lambda h: h.dma_start(
            out=gaya[ts][:], in_=yaT.rearrange("(j p) t -> p j t", p=128)[:, :, t * TT:(t + 1) * TT]),
            reads=[b_yaT[t]], writes=[b_gaya[ts]])

    def p2_head_a(t):
        for sbi in range(NSB):
            xsl = (t % 3) * NSB + sbi
            tok0 = t * TT + sbi * 128
            R.dma("sp", "xs%d" % xsl, lambda h, xsl=xsl, tok0=tok0: h.dma_start(out=xs2[xsl][:], in_=x[tok0:tok0 + 128, :]),
                  writes=[b_xs2[xsl]])
            rms_head(xs2[xsl][:], b_xs2[xsl], st2[xsl][:, 0:1], st2[xsl][:, 1:2], b_st2[xsl])
            R.op("dve", lambda h, xsl=xsl, sbi=sbi: h.tensor_scalar(out=hbB[sbi][:], in0=xs2[xsl][:], scalar1=st2[xsl][:, 1:2],
                                                                     scalar2=None, op0=ALU.mult),
                 reads=[b_xs2[xsl], b_st2[xsl]], writes=[b_hb[sbi]])
            R.op("dve", lambda h, xsl=xsl: h.tensor_tensor(out=xs2[xsl][:], in0=xs2[xsl][:], in1=Boutbc[:], op=ALU.add),
                 reads=[b_Bout], writes=[b_xs2[xsl]])

    def p2_head_b(t):
        ts = t % 2
        for sbi in range(NSB):
            R.group("pe", [(lambda h, k=k, sbi=sbi: h.transpose(out=tr2[:, k, :], in_=hbB[sbi][:, k * 128:(k + 1) * 128],
                                                               identity=identb[:])) for k in range(KC)],
                    reads=[b_hb[sbi], b_identb], writes=[b_tr2])
            R.op("act", lambda h, sbi=sbi, ts=ts: h.activation(out=hTB[ts][:, :, sbi * 128:(sbi + 1) * 128], in_=tr2[:],
                                                               func=AF.Copy),
                 reads=[b_tr2], writes=[b_hT[ts][sbi]])

    def p2_V(t):
        ts = t % 2
        for sbi in range(NSB):
            for n in range(2):
                pv, bpv = next_bank2()
                terms = [(ones_t[:, 0:128], Bbc2[:, BI["b_v"], n * 512:(n + 1) * 512], [b_Bbc, b_ones])]
                terms += [(hTB[ts][:, k, sbi * 128:(sbi + 1) * 128], Wz[:, k, D + n * 512:D + (n + 1) * 512],
                           [b_hT[ts][sbi], b_Wz[k]]) for k in range(KC)]
                mm_group(pv[:, 0:512], bpv, terms)
                R.op("act", lambda h, sbi=sbi, n=n, pv=pv: h.activation(out=v[sbi][:, n * 512:(n + 1) * 512], in_=pv[:, 0:512],
                                                                        func=AF.Gelu),
                     reads=[bpv], writes=[b_v[sbi][n]])

    def p2_U(t):
        ts = t % 2
        for j in range(KC):
            pu, bpu = next_bank2()
            terms = [(Wz[:, k, j * 128:(j + 1) * 128], hTB[ts][:, k, :], [b_Wz[k]] + b_hT[ts]) for k in range(KC)]
            mm_group(pu[:, 0:TT], bpu, terms)
            R.op("act", lambda h, j=j, pu=pu: h.activation(out=uT[:, j, :], in_=pu[:, 0:TT], func=AF.Gelu,
                                                           bias=col("b_u", j), scale=1.0),
                 reads=[bpu, b_cols], writes=[b_uT[j]])

    def p2_GB(t):
        ts = t % 2
        for jo in range(KC):
            pg, bpg = next_bank2()
            terms = [(Wgb[:, k, jo * 128:(jo + 1) * 128], hTB[ts][:, k, :], [b_Wgb[k]] + b_hT[ts]) for k in range(KC)]
            mm_group(pg[:, 0:TT], bpg, terms)
            R.op("act", lambda h, jo=jo, pg=pg: h.activation(out=tgb[:, jo, :], in_=pg[:, 0:TT], func=AF.Tanh,
                                                             bias=colh("b_gb", jo), scale=0.5),
                 reads=[bpg, b_colsh], writes=[b_tgb[jo]])

    def p2_LN(t):
        for sbi in range(NSB):
            for n in range(2):
                R.op("dve", lambda h, sbi=sbi, n=n: h.bn_stats(out=bst[sbi][:, n, :], in_=v[sbi][:, n * 512:(n + 1) * 512]),
                     reads=[b_v[sbi][n]], pwrites=[b_bst[sbi]])
            R.op("dve", lambda h, sbi=sbi: h.bn_aggr(out=mv[sbi][:, 0:2], in_=bst[sbi][:]), reads=[b_bst[sbi]], writes=[b_mv[sbi]])
            R.op("dve", lambda h, sbi=sbi: h.tensor_scalar(out=mv[sbi][:, 2:3], in0=mv[sbi][:, 1:2], scalar1=EPS, scalar2=None,
                                                           op0=ALU.add), reads=[b_mv[sbi]], writes=[b_mv[sbi]])
            R.op("pool", lambda h, sbi=sbi: h.tensor_tensor(out=mv[sbi][:, 3:4], in0=mv[sbi][:, 2:3], in1=negh[:, 0:1], op=ALU.pow),
                 reads=[b_mv[sbi], b_negh], writes=[b_mv[sbi]])
            R.op("dve", lambda h, sbi=sbi: h.tensor_scalar(out=v[sbi][:], in0=v[sbi][:], scalar1=mv[sbi][:, 0:1],
                                                           scalar2=mv[sbi][:, 3:4], op0=ALU.subtract, op1=ALU.mult),
                 reads=[b_mv[sbi]], writes=b_v[sbi])
            R.op("dve", lambda h, sbi=sbi: h.tensor_tensor(out=vn[sbi][:], in0=v[sbi][:], in1=Gbc[:], op=ALU.mult),
                 reads=b_v[sbi] + [b_Gbc], writes=[b_vn[sbi]])

    def p2_SGmm(t):
        for sbi in range(NSB):
            bg, bbg = next_big()
            R.group("pe", [(lambda h, hd=hd, sbi=sbi, bg=bg: h.matmul(bg[:, hd, :], lhsT=vn[sbi][:, hd * 128:(hd + 1) * 128],
                                                                      rhs=WmT[:, hd, :], start=True, stop=True))
                           for hd in range(8)], reads=[b_vn[sbi], b_WmT], writes=[bbg])
            R.op("dve", lambda h, sbi=sbi, bg=bg: h.tensor_tensor(out=tmpS[sbi][:], in0=bg[:], in1=Cst[:], op=ALU.add),
                 reads=[bbg, b_Cst], writes=[b_tmpS[sbi]])
            R.op("dve", lambda h, sbi=sbi: h.tensor_tensor(out=sT[:, :, sbi * 128:(sbi + 1) * 128], in0=tmpS[sbi][:],
                                                            in1=uT[:, :, sbi * 128:(sbi + 1) * 128], op=ALU.mult),
                 reads=[b_tmpS[sbi]] + b_uT, writes=[b_sT[sbi]])

    def p2_SGO(t):
        ts = t % 2
        for jo in range(KC):
            pb, bpb = next_bank2()
            terms = [(Bbc2[:, BI["b_sgo"], jo * 128:(jo + 1) * 128], ones_t[:, 0:TT], [b_Bbc, b_ones])]
            terms += [(Wsgo[:, k, jo * 128:(jo + 1) * 128], sT[:, k, :], [b_Wsgo[k]] + b_sT) for k in range(KC)]
            mm_group(pb[:, 0:TT], bpb, terms)
            s2 = jo % 2
            R.op("dve", lambda h, jo=jo, pb=pb, s2=s2: h.scalar_tensor_tensor(out=t1[s2][:], in0=tgb[:, jo, :], scalar=1.0,
                                                                              in1=pb[:, 0:TT], op0=ALU.add, op1=ALU.mult),
                 reads=[b_tgb[jo], bpb], writes=[b_t1[s2]])
            R.op("dve", lambda h, jo=jo, s2=s2: h.tensor_tensor(out=mixT[:, jo, :], in0=t1[s2][:], in1=gaya[ts][:, jo, :],
                                                                 op=ALU.add),
                 reads=[b_t1[s2], b_gaya[ts]], writes=[b_mixT[jo]])

    def p2_OUT(t):
        for sbi in range(NSB):
            xsl = (t % 3) * NSB + sbi
            for n in range(2):
                px, bpx = next_bank2()
                terms = [(mixT[:, k, sbi * 128:(sbi + 1) * 128], Wout[:, k, n * 512:(n + 1) * 512], [b_mixT[k], b_Wout[k]])
                         for k in range(KC)]
                mm_group(px[:, 0:512], bpx, terms)
                R.op("dve", lambda h, xsl=xsl, n=n, px=px: h.tensor_tensor(out=xs2[xsl][:, n * 512:(n + 1) * 512], in0=px[:, 0:512],
                                                                           in1=xs2[xsl][:, n * 512:(n + 1) * 512], op=ALU.add),
                     reads=[bpx], writes=[b_xs2[xsl]])

    def p2_route(gsb, s, pl):
        r = rt[s]
        B = lambda n: r["b_" + n]
        R.op("dve", lambda h: h.tensor_copy(out=r["L"][:], in_=pl[0][:, 0:36]), reads=[pl[1]], writes=[B("L")])
        R.op("dve", lambda h: h.tensor_copy(out=r["gl8"][:, 0:4], in_=r["L"][:, 0:4]), reads=[B("L")], writes=[B("gl8")])
        R.op("dve", lambda h: h.max(out=r["t8g"][:], in_=r["gl8"][:]), reads=[B("gl8")], writes=[B("t8g")])
        R.op("dve", lambda h: h.max_index(out=r["i8g"][:], in_max=r["t8g"][:], in_values=r["gl8"][:]),
             reads=[B("gl8"), B("t8g")], writes=[B("i8g")])
        R.op("dve", lambda h: h.tensor_scalar(out=GLM[:, gsb, :], in0=r["L"][:, 0:4], scalar1=r["t8g"][:, 0:1], scalar2=None,
                                              op0=ALU.subtract), reads=[B("L"), B("t8g")], pwrites=[b_GLM])
        R.op("dve", lambda h: h.tensor_copy(out=r["gself"][:], in_=r["i8g"][:, 0:1]), reads=[B("i8g")], writes=[B("gself")])
        R.op("dve", lambda h: h.tensor_scalar(out=r["pen32"][:], in0=iotg32[:], scalar1=r["gself"][:, 0:1], scalar2=None,
                                              op0=ALU.is_equal), reads=[B("gself"), b_rc], writes=[B("pen32")])
        R.op("dve", lambda h: h.tensor_scalar(out=r["pen32"][:], in0=r["pen32"][:], scalar1=1.0, scalar2=1e30,
                                              op0=ALU.subtract, op1=ALU.mult), reads=[B("pen32")], writes=[B("pen32")])
        R.op("dve", lambda h: h.tensor_tensor(out=r["M32"][:], in0=r["L"][:, 4:36], in1=r["pen32"][:], op=ALU.add),
             reads=[B("L"), B("pen32")], writes=[B("M32")])
        R.op("dve", lambda h: h.max(out=r["t8e"][:], in_=r["M32"][:]), reads=[B("M32")], writes=[B("t8e")])
        R.op("dve", lambda h: h.max_index(out=r["i8e"][:], in_max=r["t8e"][:], in_values=r["M32"][:]),
             reads=[B("M32"), B("t8e")], writes=[B("i8e")])
        R.op("dve", lambda h: h.tensor_tensor(out=DV[:, gsb:gsb + 1], in0=r["t8e"][:, 0:1], in1=r["t8e"][:, 1:2], op=ALU.subtract),
             reads=[B("t8e")], pwrites=[b_DV])
        R.op("dve", lambda h: h.tensor_copy(out=r["ef"][:], in_=r["i8e"][:, 0:2]), reads=[B("i8e")], writes=[B("ef")])
        R.op("dve", lambda h: h.tensor_scalar(out=r["A0"][:], in0=iotx32[:], scalar1=r["ef"][:, 0:1], scalar2=None,
                                              op0=ALU.is_equal), reads=[B("ef"), b_rc], writes=[B("A0")])
        R.op("dve", lambda h: h.tensor_scalar(out=r["A1"][:], in0=iotx32[:], scalar1=r["ef"][:, 1:2], scalar2=None,
                                              op0=ALU.is_equal), reads=[B("ef"), b_rc], writes=[B("A1")])
        R.op("dve", lambda h: h.tensor_tensor(out=r["Ab"][:], in0=r["A0"][:], in1=r["A1"][:], op=ALU.add),
             reads=[B("A0"), B("A1")], writes=[B("Ab")])

    def p2_route_b(gsb, s):
        r = rt[s]
        B = lambda n: r["b_" + n]
        ps, bps = next_bank2()
        tt_ = gsb // NSB
        sbi_ = gsb % NSB
        a_in, a_out = tt_ % 2, (tt_ + 1) % 2
        terms = [(Ustrict[:], r["Ab"][:], [b_rc, B("Ab")]), (ones_t[:, 0:128], Acum[a_in][:], [b_ones, b_Acum[a_in]])]
        for pj in range(sbi_):
            rp = rt[(gsb - sbi_ + pj) % 4]
            terms.append((ones_t[:, 0:128], rp["Ab"][:], [b_ones, rp["b_Ab"]]))
        mm_group(ps[:, 0:32], bps, terms)
        if sbi_ == NSB - 1:
            for pj in range(NSB):
                rp = rt[(gsb - sbi_ + pj) % 4]
                src = Acum[a_in] if pj == 0 else Acum[a_out]
                bsrc = b_Acum[a_in] if pj == 0 else b_Acum[a_out]
                R.op("dve", lambda h, rp=rp, src=src: h.tensor_tensor(out=Acum[a_out][:], in0=src[:], in1=rp["Ab"][:], op=ALU.add),
                     reads=[bsrc, rp["b_Ab"]], writes=[b_Acum[a_out]])
        for kk, An in enumerate(["A0", "A1"]):
            R.op("dve", lambda h, An=An: h.tensor_tensor(out=r["prod"][:], in0=r[An][:], in1=ps[:, 0:32], op=ALU.mult),
                 reads=[B(An), bps], writes=[B("prod")])
            R.op("dve", lambda h, kk=kk: h.tensor_reduce(out=r["pos"][:, kk:kk + 1], in_=r["prod"][:], axis=AX.X, op=ALU.add),
                 reads=[B("prod")], pwrites=[B("pos")])
        R.op("dve", lambda h: h.tensor_scalar(out=r["pos"][:], in0=r["pos"][:], scalar1=float(CAP - 1), scalar2=None, op0=ALU.min),
             reads=[B("pos")], writes=[B("pos")])
        R.op("dve", lambda h: h.scalar_tensor_tensor(out=r["dest"][:], in0=r["ef"][:], scalar=float(CAP), in1=r["pos"][:],
                                                     op0=ALU.mult, op1=ALU.add), reads=[B("ef"), B("pos")], writes=[B("dest")])
        R.op("dve", lambda h: h.tensor_copy(out=DEST[:, gsb, :], in_=r["dest"][:]), reads=[B("dest")], writes=[b_DEST[gsb]])
        for kk in range(2):
            R.dma("pool", "sc%d_%d" % (s, kk), lambda h, kk=kk: h.indirect_dma_start(
                out=xpad[:, :], out_offset=bass.IndirectOffsetOnAxis(ap=DEST[:, gsb, kk:kk + 1], axis=0),
                in_=h2b[s][:], in_offset=None), reads=[b_h2b[s], b_DEST[gsb]], pwrites=[b_xpad])

    def p2_tail(t):
        for sbi in range(NSB):
            xsl = (t % 3) * NSB + sbi
            gsb = t * NSB + sbi
            s = gsb % 2
            R.dma("sp", "x1s%d" % xsl, lambda h, xsl=xsl, gsb=gsb: h.dma_start(out=x1s[gsb * 128:(gsb + 1) * 128, :], in_=xs2[xsl][:]),
                  reads=[b_xs2[xsl]], writes=[b_x1s[gsb]])
            rms_head(xs2[xsl][:], b_xs2[xsl], st2[xsl][:, 2:3], st2[xsl][:, 3:4], b_st2[xsl])
            R.op("dve", lambda h, xsl=xsl, s=s: h.tensor_scalar(out=h2f[s][:], in0=xs2[xsl][:], scalar1=st2[xsl][:, 3:4],
                                                                 scalar2=None, op0=ALU.mult),
                 reads=[b_xs2[xsl], b_st2[xsl]], writes=[b_h2f[s]])
            R.op("act", lambda h, xsl=xsl, gsb=gsb: h.activation(out=h2b[gsb % 4][:], in_=xs2[xsl][:], func=AF.Identity,
                                                                 scale=st2[xsl][:, 3:4]),
                 reads=[b_xs2[xsl], b_st2[xsl]], writes=[b_h2b[gsb % 4]])

    def p2_tail_b(t):
        pls = []
        for sbi in range(NSB):
            gsb = t * NSB + sbi
            s = gsb % 2
            bg, bbg = next_big()
            R.group("pe", [(lambda h, k=k, s=s, bg=bg: h.transpose(out=bg[:, k, :], in_=h2f[s][:, k * 128:(k + 1) * 128],
                                                                   identity=identf[:])) for k in range(KC)],
                    reads=[b_h2f[s], b_identf], writes=[bbg])
            R.op("act", lambda h, s=s, bg=bg: h.activation(out=h2T[s][:], in_=bg[:], func=AF.Copy), reads=[bbg], writes=[b_h2T[s]])
            pl = next_bank2()
            terms = [(onesf[:], rbbc[:, 0:36], [b_ones, b_rows])]
            terms += [(h2T[s][:, k, :], Wr[:, k, :], [b_h2T[s], b_Wr]) for k in range(KC)]
            mm_group(pl[0][:, 0:36], pl[1], terms)
            pls.append(pl)
        for sbi in range(NSB):
            gsb = t * NSB + sbi
            p2_route(gsb, gsb % 4, pls[sbi])

    p2_gaya(0)
    p2_head_a(0)
    p2_head_b(0)
    if NT > 1:
        p2_head_a(1)
    p2_V(0)
    p2_LN(0)
    p2_U(0)
    p2_GB(0)
    for t in range(NT):
        nxt = t + 1 < NT
        p2_SGmm(t)
        if nxt:
            p2_head_b(t + 1)
            p2_V(t + 1)
        p2_SGO(t)
        if nxt:
            p2_LN(t + 1)
            p2_gaya(t + 1)
            p2_U(t + 1)
        p2_OUT(t)
        p2_tail(t)
        if nxt:
            p2_GB(t + 1)
        p2_tail_b(t)
        if t + 2 < NT:
            p2_head_a(t + 2)
        if t > 0:
            for sbi in range(NSB):
                p2_route_b((t - 1) * NSB + sbi, ((t - 1) * NSB + sbi) % 4)
    for sbi in range(NSB):
        p2_route_b((NT - 1) * NSB + sbi, ((NT - 1) * NSB + sbi) % 4)
    R.op("act", lambda h: h.activation(out=GLM[:], in_=GLM[:], func=AF.Exp), reads=[b_GLM], writes=[b_GLM])
    R.op("dve", lambda h: h.tensor_reduce(out=EW1[:], in_=GLM[:], axis=AX.X, op=ALU.add), reads=[b_GLM], writes=[b_EW])
    R.op("dve", lambda h: h.reciprocal(out=EW1[:], in_=EW1[:]), reads=[b_EW], writes=[b_EW])
    R.op("act", lambda h: h.activation(out=EW0[:], in_=DV[:], func=AF.Sigmoid), reads=[b_DV, b_EW], writes=[b_EW])
    R.op("dve", lambda h: h.tensor_tensor(out=EW0[:], in0=EW0[:], in1=EW1[:], op=ALU.mult), reads=[b_EW], writes=[b_EW])
    R.op("dve", lambda h: h.tensor_tensor(out=EW1[:], in0=EW1[:], in1=EW0[:], op=ALU.subtract), reads=[b_EW], writes=[b_EW])
    if debug:
        R.dma("sp", "dbg0", lambda h: h.dma_start(out=dbg_dest[:, :], in_=DEST[:].rearrange("p a b -> p (a b)")), reads=b_DEST)
        R.dma("sp", "dbg1", lambda h: h.dma_start(out=dbg_ew[:, 0:NGSB], in_=EW0[:]), reads=[b_EW])
        R.dma("sp", "dbg2", lambda h: h.dma_start(out=dbg_ew[:, NGSB:2 * NGSB], in_=EW1[:]), reads=[b_EW])
    barrier()
    A2.free()

    A3 = Alloc(nc)
    g2c = cols
    stg_g = A3.sb("stg_g", [128, KC, 512], F32)
    stg_u = A3.sb("stg_u", [128, KC, 512], F32)
    stg_d = A3.sb("stg_d", [128, 4, D], F32)
    Wg_b = [A3.sb("Wg_b%d" % i, [128, KC, 512], BF16) for i in range(2)]
    Wu_b = [A3.sb("Wu_b%d" % i, [128, KC, 512], BF16) for i in range(2)]
    Wd_b = [A3.sb("Wd_b%d" % i, [128, 4, D], BF16) for i in range(2)]
    xp = [A3.sb("xp%d" % i, [128, NB, D], BF16) for i in range(2)]
    xT = A3.sb("xT", [128, KC, CAP], BF16)
    sgt = [A3.sb("sgt%d" % i, [128, 512], F32) for i in range(2)]
    hTe = A3.sb("hTe", [128, 4, CAP], BF16)
    NYB = 8
    ybuf = [A3.sb("ybuf%d" % i, [128, D], F32) for i in range(NYB)]
    trE = [A3.ps("trE%d" % i, [128, KC, 128], BF16) for i in range(2)]
    mmE = [A3.ps("mmE%d" % i, [128, 512], F32) for i in range(6)]
    b_stg = {n: Buf("stg_" + n) for n in "gud"}
    b_We = [{n: Buf("W%s%d" % (n, i)) for n in "gud"} for i in range(2)]
    b_xp = [Buf("xp0"), Buf("xp1")]
    b_xT = [Buf("xT%d" % b) for b in range(NB)]
    b_sgt = [Buf("sgt0"), Buf("sgt1")]
    b_hTe = [Buf("hTe%d" % i) for i in range(8)]
    b_ybuf = [Buf("ybuf%d" % i) for i in range(NYB)]
    b_trE = [Buf("trE0"), Buf("trE1")]
    b_mmE = [Buf("mmE%d" % i) for i in range(6)]
    b_ypad = [Buf("ypad%d" % e) for e in range(NE)]
    _me = [0]

    def next_bankE():
        _me[0] = (_me[0] + 1) % len(mmE)
        return mmE[_me[0]], b_mmE[_me[0]]

    halves = []
    r0 = 0
    while r0 < CAP:
        r1 = min(CAP, r0 + 512)
        halves.append((r0, r1))
        r0 = r1
    _yb = [0]

    def dma_g(e):
        R.dma("sp", "stg_g", lambda h: h.dma_start(out=stg_g[:], in_=w_gate[e].rearrange("(k p) f -> p k f", p=128)),
              writes=[b_stg["g"]])

    def dma_u(e):
        R.dma("sp", "stg_u", lambda h: h.dma_start(out=stg_u[:], in_=w_up[e].rearrange("(k p) f -> p k f", p=128)),
              writes=[b_stg["u"]])

    def dma_d(e):
        R.dma("sp", "stg_d", lambda h: h.dma_start(out=stg_d[:], in_=w_down[e].rearrange("(k p) f -> p k f", p=128)),
              writes=[b_stg["d"]])

    def dma_xp(e):
        sl = e % 2
        R.dma("sp", "xp%d" % sl, lambda h: h.dma_start(
            out=xp[sl][:], in_=xpad[e * CAP:(e + 1) * CAP, :].rearrange("(b p) d -> p b d", p=128)),
            reads=[b_xpad], writes=[b_xp[sl]])

    def cast_g(e, k):
        sl = e % 2
        cast_op("act", Wg_b[sl][:, k, :], stg_g[:, k, :], col("g2", k), [b_stg["g"], b_cols], [b_We[sl]["g"]])

    def cast_u(e, k):
        sl = e % 2
        cast_op("act" if k % 2 else "dve", Wu_b[sl][:, k, :], stg_u[:, k, :], col("g2", k), [b_stg["u"], b_cols], [b_We[sl]["u"]])

    def cast_d(e, m):
        sl = e % 2
        cast_op("dve", Wd_b[sl][:, m, :], stg_d[:, m, :], None, [b_stg["d"]], [b_We[sl]["d"]])

    def moe_thunks(e):
        L = []
        if e + 2 < NE:
            L.append(lambda: dma_xp(e + 2))
        if e + 1 < NE:
            L += [(lambda k=k: cast_g(e + 1, k)) for k in range(KC)]
        if e + 2 < NE:
            L.append(lambda: dma_g(e + 2))
        if e + 1 < NE:
            L += [(lambda k=k: cast_u(e + 1, k)) for k in range(KC)]
        if e + 2 < NE:
            L.append(lambda: dma_u(e + 2))
        if e + 1 < NE:
            L += [(lambda m=m: cast_d(e + 1, m)) for m in range(4)]
        if e + 2 < NE:
            L.append(lambda: dma_d(e + 2))
        return L

    def moe_compute_a(e, L):
        sl = e % 2
        for blk in range(NB):
            ti = blk % 2
            R.group("pe", [(lambda h, k=k, blk=blk, ti=ti: h.transpose(out=trE[ti][:, k, :], in_=xp[sl][:, blk, k * 128:(k + 1) * 128],
                                                                      identity=identb[:])) for k in range(KC)],
                    reads=[b_xp[sl], b_identb], writes=[b_trE[ti]])
            if blk % 2 == 0:
                R.op("act", lambda h, blk=blk, ti=ti: h.activation(out=xT[:, :, blk * 128:(blk + 1) * 128], in_=trE[ti][:], func=AF.Copy),
                     reads=[b_trE[ti]], writes=[b_xT[blk]])
            else:
                R.op("dve", lambda h, blk=blk, ti=ti: h.tensor_copy(out=xT[:, :, blk * 128:(blk + 1) * 128], in_=trE[ti][:]),
                     reads=[b_trE[ti]], writes=[b_xT[blk]])
        for hi, (r0, r1) in enumerate(halves):
            n = r1 - r0
            xbufs = [b_xT[b] for b in range(r0 // 128, (r1 + 127) // 128)]
            for m in range(4):
                pg, bpg = next_bankE()
                mm_group(pg[:, 0:n], bpg, [(Wg_b[sl][:, k, m * 128:(m + 1) * 128], xT[:, k, r0:r1], [b_We[sl]["g"]] + xbufs)
                                           for k in range(KC)])
                pu, bpu = next_bankE()
                mm_group(pu[:, 0:n], bpu, [(Wu_b[sl][:, k, m * 128:(m + 1) * 128], xT[:, k, r0:r1], [b_We[sl]["u"]] + xbufs)
                                           for k in range(KC)])
                s2 = m % 2
                R.op("act", lambda h, pg=pg, s2=s2, n=n: h.activation(out=sgt[s2][:, 0:n], in_=pg[:, 0:n], func=AF.Silu),
                     reads=[bpg], writes=[b_sgt[s2]])
                R.op("dve", lambda h, pu=pu, s2=s2, n=n, m=m, r0=r0, r1=r1: h.tensor_tensor(
                    out=hTe[:, m, r0:r1], in0=sgt[s2][:, 0:n], in1=pu[:, 0:n], op=ALU.mult),
                    reads=[b_sgt[s2], bpu], writes=[b_hTe[hi * 4 + m]])
                if L:
                    L.pop(0)()

    def moe_compute_b(e, L):
        sl = e % 2
        for blk in range(NB):
            hi = (blk * 128) // 512
            _yb[0] = (_yb[0] + 1) % NYB
            ys = _yb[0]
            for n2 in range(2):
                py, bpy = next_bankE()
                mm_group(py[:, 0:512], bpy, [(hTe[:, m, blk * 128:(blk + 1) * 128], Wd_b[sl][:, m, n2 * 512:(n2 + 1) * 512],
                                              [b_hTe[hi * 4 + m], b_We[sl]["d"]]) for m in range(4)])
                if n2 == 0:
                    R.op("act", lambda h, py=py, ys=ys: h.activation(out=ybuf[ys][:, 0:512], in_=py[:, 0:512], func=AF.Copy),
                         reads=[bpy], writes=[b_ybuf[ys]])
                else:
                    R.op("dve", lambda h, py=py, ys=ys: h.tensor_copy(out=ybuf[ys][:, 512:1024], in_=py[:, 0:512]),
                         reads=[bpy], pwrites=[b_ybuf[ys]])
                if L:
                    L.pop(0)()
            R.dma("pool", "yst%d" % ys, lambda h, blk=blk, ys=ys: h.dma_start(
                out=ypad[e * CAP + blk * 128:e * CAP + (blk + 1) * 128, :], in_=ybuf[ys][:]),
                reads=[b_ybuf[ys]], pwrites=[b_ypad[e]])

    dma_xp(0); dma_g(0); dma_u(0); dma_d(0)
    for k in range(KC):
        cast_g(0, k)
    dma_g(1)
    for k in range(KC):
        cast_u(0, k)
    dma_u(1)
    for m in range(4):
        cast_d(0, m)
    dma_d(1)
    dma_xp(1)
    for e in range(NE):
        L = moe_thunks(e)
        moe_compute_a(e, L)
        moe_compute_b(e, L)
        while L:
            L.pop(0)()
    barrier()
    A3.free()

    A4 = Alloc(nc)
    Gfbc = A4.sb("Gfbc", [128, D], F32)
    GRP = 4
    NCB = 2 * GRP
    x1t = [A4.sb("x1t%d" % i, [128, D], F32) for i in range(NCB)]
    y0 = [A4.sb("y0_%d" % i, [128, D], F32) for i in range(NCB)]
    y1 = [A4.sb("y1_%d" % i, [128, D], F32) for i in range(NCB)]
    st3 = [A4.sb("st3_%d" % i, [128, 2, GRP], F32) for i in range(2)]
    b_Gf = Buf("Gf")
    b_x1t = [Buf("x1t%d" % i) for i in range(NCB)]
    b_y0 = [Buf("y0_%d" % i) for i in range(NCB)]
    b_y1 = [Buf("y1_%d" % i) for i in range(NCB)]
    b_st3 = [Buf("st3_%d" % i) for i in range(2)]
    R.dma("sp", "c9", lambda h: h.dma_start(out=Gfbc[:], in_=bvecs_d[BV["g_f"]:BV["g_f"] + 1, :].partition_broadcast(128)), writes=[b_Gf])

    def cmb_load(gsb):
        s = gsb % NCB
        R.dma("sp", "x1t%d" % s, lambda h: h.dma_start(out=x1t[s][:], in_=x1s[gsb * 128:(gsb + 1) * 128, :]),
              reads=[b_x1s[gsb]], writes=[b_x1t[s]])
        R.dma("pool", "g0_%d" % s, lambda h: h.indirect_dma_start(
            out=y0[s][:], out_offset=None, in_=ypad[:, :],
            in_offset=bass.IndirectOffsetOnAxis(ap=DEST[:, gsb, 0:1], axis=0)), reads=b_ypad + [b_DEST[gsb]], writes=[b_y0[s]])
        R.dma("pool", "g1_%d" % s, lambda h: h.indirect_dma_start(
            out=y1[s][:], out_offset=None, in_=ypad[:, :],
            in_offset=bass.IndirectOffsetOnAxis(ap=DEST[:, gsb, 1:2], axis=0)), reads=b_ypad + [b_DEST[gsb]], writes=[b_y1[s]])

    def cmb_group(g):
        gs = list(range(g * GRP, min(NGSB, (g + 1) * GRP)))
        sg = g % 2
        for i, gsb in enumerate(gs):
            s = gsb % NCB
            R.op("dve", lambda h, s=s, gsb=gsb: h.scalar_tensor_tensor(out=x1t[s][:], in0=y0[s][:], scalar=EW0[:, gsb:gsb + 1],
                                                                       in1=x1t[s][:], op0=ALU.mult, op1=ALU.add),
                 reads=[b_y0[s], b_EW], writes=[b_x1t[s]])
            R.op("dve", lambda h, s=s, gsb=gsb: h.scalar_tensor_tensor(out=x1t[s][:], in0=y1[s][:], scalar=EW1[:, gsb:gsb + 1],
                                                                       in1=x1t[s][:], op0=ALU.mult, op1=ALU.add),
                 reads=[b_y1[s], b_EW], writes=[b_x1t[s]])
            _jk[0] ^= 1
            jk = _jk[0]
            R.op("act", lambda h, s=s, i=i, jk=jk: h.activation(out=junk[jk][:], in_=x1t[s][:], func=AF.Square,
                                                                accum_out=st3[sg][:, 0, i:i + 1]),
                 reads=[b_x1t[s]], writes=[b_junk[jk]], pwrites=[b_st3[sg]])
        R.op("dve", lambda h: h.tensor_scalar(out=st3[sg][:, 0, :], in0=st3[sg][:, 0, :], scalar1=1.0 / D, scalar2=EPS,
                                              op0=ALU.mult, op1=ALU.add), reads=[b_st3[sg]], writes=[b_st3[sg]])
        R.op("pool", lambda h: h.tensor_tensor(out=st3[sg][:, 1, :], in0=st3[sg][:, 0, :], in1=negh[:, 0:GRP], op=ALU.pow),
             reads=[b_st3[sg], b_negh], writes=[b_st3[sg]])
        for i, gsb in enumerate(gs):
            s = gsb % NCB
            R.op("dve", lambda h, s=s, i=i: h.scalar_tensor_tensor(out=x1t[s][:], in0=x1t[s][:], scalar=st3[sg][:, 1, i:i + 1],
                                                                   in1=Gfbc[:], op0=ALU.mult, op1=ALU.mult),
                 reads=[b_st3[sg], b_Gf], writes=[b_x1t[s]])
            R.dma("sp", "ot%d" % s, lambda h, s=s, gsb=gsb: h.dma_start(out=out[gsb * 128:(gsb + 1) * 128, :], in_=x1t[s][:]),
                  reads=[b_x1t[s]])

    NG = (NGSB + GRP - 1) // GRP
    for gsb in range(min(GRP, NGSB)):
        cmb_load(gsb)
    for g in range(NG):
        for gsb in range((g + 1) * GRP, min(NGSB, (g + 2) * GRP)):
            cmb_load(gsb)
        cmb_group(g)
    barrier()
    R.emit()
    A4.free()
    PA.free()
    R.close()
    return nc


_NC_CACHE = {}


def _layout_inputs(inp):
    f = lambda a: np.ascontiguousarray(np.asarray(a, dtype=np.float32))
    vec8 = lambda v: f(np.asarray(v).reshape(8, 128).T)
    b_in = np.asarray(inp["b_in"])[0]
    colmap = {
        "g1": inp["norm_mix_g"][0], "b_a2": b_in[1024:2048], "conv_b": inp["conv_b"][0], "ln_g": inp["conv_ln_g"][0],
        "ln_b": inp["conv_ln_b"][0], "b_u": b_in[2048:3072], "b_ga": b_in[4096:5120], "b_gb": b_in[5120:6144],
        "sg_b": inp["sg_ln_b"][0], "g2": inp["norm_ffn_g"][0],
    }
    cols = f(np.concatenate([vec8(colmap[n]) for n in COLV], axis=1))
    convwT = f(np.asarray(inp["conv_w"])[0].reshape(31, 8, 128).transpose(2, 1, 0).reshape(128, 8 * 31))
    wsgT = f(np.asarray(inp["w_sg"])[0].transpose(2, 0, 1).reshape(128, 8 * 128))
    blk = np.arange(128) // 64
    maskT = f((blk[None, :] >= blk[:, None]).astype(np.float32))
    rb = f(np.concatenate([np.asarray(inp["b_router_group"])[0], np.asarray(inp["b_router_expert"])[0]])[None, :])
    bvecs = f(np.stack([np.asarray(inp["sg_ln_g"])[0], np.asarray(inp["b_out"])[0], np.asarray(inp["norm_final_g"]),
                        np.asarray(inp["b_sg"])[0].reshape(-1), b_in[0:1024], np.asarray(inp["b_conv_out"])[0],
                        np.asarray(inp["b_sg_out"])[0], b_in[3072:4096]]))
    wr = f(np.concatenate([np.asarray(inp["w_router_group"])[0], np.asarray(inp["w_router_expert"])[0]], axis=1))
    shared = {
        "w_in": f(inp["w_in"][0]), "w_co": f(inp["w_conv_out"][0]), "w_sgo": f(inp["w_sg_out"][0]), "w_out": f(inp["w_out"][0]),
        "w_gate": f(inp["w_expert_gate"][0]), "w_up": f(inp["w_expert_up"][0]), "w_down": f(inp["w_expert_down"][0]),
        "cols": cols, "convwT": convwT, "wsgT": wsgT, "maskT": maskT, "rb": rb, "bvecs": bvecs, "wr": wr,
    }
    return shared


def kernel(**inputs):
    x = np.asarray(inputs["x"], dtype=np.float32)
    B, S, Dm = x.shape
    ncores = 8
    nseq = B // ncores
    key = (nseq, S)
    if key not in _NC_CACHE:
        _NC_CACHE[key] = build_nc(NSEQ=nseq, S=S, NB=8)
    nc = _NC_CACHE[key]
    shared = _layout_inputs(inputs)
    in_maps = []
    for c in range(ncores):
        m = dict(shared)
        m["x"] = np.ascontiguousarray(x[c * nseq:(c + 1) * nseq].reshape(nseq * S, Dm))
        in_maps.append(m)
    res = run_bass_kernel_spmd(nc, in_maps, core_ids=list(range(ncores)))
    outs = [np.asarray(r["out"]).reshape(nseq, S, Dm) for r in res.results]
    return np.concatenate(outs, axis=0).astype(np.float32)
```

```python
import numpy as np
import concourse.bass as bass
import concourse.mybir as mybir
from concourse.bass_utils import run_bass_kernel_spmd

F32 = mybir.dt.float32
BF16 = mybir.dt.bfloat16
I32 = mybir.dt.int32
U32 = mybir.dt.uint32
ALU = mybir.AluOpType
AF = mybir.ActivationFunctionType
AX = mybir.AxisListType


class Buf:
    __slots__ = ("name", "w", "r")

    def __init__(self, name=""):
        self.name = name
        self.w = {}
        self.r = {}


class Rec:
    ENGS = ("pe", "act", "dve", "pool", "sp")

    def __init__(self, nc):
        self.nc = nc
        self.prog = {e: [] for e in self.ENGS}
        self.sem = {}
        self.cnt = {e: 0 for e in self.ENGS}
        self.seen = {e: {} for e in self.ENGS}
        self.dsem = {}
        self._ctx = []
        for e in self.ENGS:
            cm = nc.semaphore("s_" + e)
            self.sem[e] = cm.__enter__()
            self._ctx.append(cm)

    def dma_sem(self, key):
        if key not in self.dsem:
            cm = self.nc.semaphore("d_%s" % (key,))
            s = cm.__enter__()
            self._ctx.append(cm)
            self.dsem[key] = [s, 0]
        return self.dsem[key]

    def _deps(self, eng, reads, writes, pwrites):
        need = {}
        def add(d):
            for k, (s, v) in d.items():
                if k not in need or need[k][1] < v:
                    need[k] = (s, v)
        for b in reads:
            add(b.w)
        for b in writes:
            add(b.w); add(b.r)
        for b in pwrites:
            add(b.w); add(b.r)
        waits = []
        seen = self.seen[eng]
        for k, (s, v) in need.items():
            if seen.get(k, 0) < v:
                seen[k] = v
                waits.append((s, v))
        return waits

    def _publish(self, ev, reads, writes, pwrites):
        k = id(ev[0])
        for b in reads:
            b.r[k] = ev
        for b in writes:
            b.w = {k: ev}; b.r = {}
        for b in pwrites:
            b.w[k] = ev

    def op(self, eng, fn, reads=(), writes=(), pwrites=(), inc=True):
        waits = self._deps(eng, reads, writes, pwrites)
        if inc:
            self.cnt[eng] += 1
            ev = (self.sem[eng], self.cnt[eng])
            self._publish(ev, reads, writes, pwrites)
        self.prog[eng].append((waits, fn, (self.sem[eng], 1) if inc else None))

    def group(self, eng, fns, reads=(), writes=(), pwrites=(), per_reads=None):
        n = len(fns)
        if per_reads is None:
            wl = [self._deps(eng, reads, writes, pwrites)] + [[] for _ in range(n - 1)]
            allreads = list(reads)
        else:
            wl = []
            allreads = []
            for i in range(n):
                wl.append(self._deps(eng, per_reads[i], writes if i == 0 else (), pwrites if i == 0 else ()))
                for b in per_reads[i]:
                    if b not in allreads:
                        allreads.append(b)
        self.cnt[eng] += 1
        ev = (self.sem[eng], self.cnt[eng])
        self._publish(ev, allreads, writes, pwrites)
        for i, fn in enumerate(fns):
            self.prog[eng].append((wl[i], fn, (self.sem[eng], 1) if i == n - 1 else None))

    def dma(self, eng, key, fn, reads=(), writes=(), pwrites=()):
        waits = self._deps(eng, reads, writes, pwrites)
        d = self.dma_sem(key)
        d[1] += 16
        ev = (d[0], d[1])
        self._publish(ev, reads, writes, pwrites)
        self.prog[eng].append((waits, fn, (d[0], 16)))
        return ev

    def wait_all(self, eng, bufs):
        waits = self._deps(eng, bufs, (), ())
        self.prog[eng].append((waits, None, None))

    def emit(self):
        nc = self.nc
        handles = {"pe": "tensor", "act": "scalar", "dve": "vector", "pool": "gpsimd", "sp": "sync"}
        with nc.Block() as block:
            for e in self.ENGS:
                prog = self.prog[e]

                def body(h, prog=prog):
                    for waits, fn, inc in prog:
                        for (s, v) in waits:
                            h.wait_ge(s, v)
                        if fn is None:
                            continue
                        ins = fn(h)
                        if inc is not None:
                            ins.then_inc(inc[0], inc[1])
                getattr(block, handles[e])(body)

    def close(self):
        for cm in reversed(self._ctx):
            cm.__exit__(None, None, None)


D = 1024
KC = 8
TT = 256
NSB = TT // 128
NE = 32
EPS = 1e-6
COLV = ["g1", "b_a2", "conv_b", "ln_g", "ln_b", "b_u", "b_ga", "b_gb", "sg_b", "g2"]
CI = {n: 8 * i for i, n in enumerate(COLV)}
NCOL = 8 * len(COLV)
BV = {"sg_g": 0, "b_out": 1, "g_f": 2, "b_sg": 3, "b_a1": 4, "b_co": 5, "b_sgo": 6, "b_v": 7}
BI = {"b_a1": 0, "b_co": 1, "b_sgo": 0, "b_v": 1}


class Alloc:
    def __init__(self, nc):
        self.nc = nc
        self.stack = []

    def sb(self, name, shape, dt):
        cm = self.nc.sbuf_tensor("sb_" + name, shape, dt)
        t = cm.__enter__()
        self.stack.append(cm)
        return t

    def ps(self, name, shape, dt):
        cm = self.nc.psum_tensor("ps_" + name, shape, dt)
        t = cm.__enter__()
        self.stack.append(cm)
        return t

    def free(self):
        for cm in reversed(self.stack):
            cm.__exit__(None, None, None)
        self.stack = []


def build_nc(NSEQ=2, S=4096, NB=8, debug=False):
    T = NSEQ * S
    NT = T // TT
    TPS = S // TT
    NGSB = T // 128
    CAP = NB * 128
    NROWS = NE * CAP
    nc = bass.Bass("TRN2", target_bir_lowering=False)
    dt_in = lambda name, shape: nc.dram_tensor(name, shape, F32, kind="ExternalInput").ap()
    x = dt_in("x", [T, D])
    w_in = dt_in("w_in", [D, 6 * D])
    w_co = dt_in("w_co", [D, D])
    w_sgo = dt_in("w_sgo", [D, D])
    w_out = dt_in("w_out", [D, D])
    w_gate = dt_in("w_gate", [NE, D, 512])
    w_up = dt_in("w_up", [NE, D, 512])
    w_down = dt_in("w_down", [NE, 512, D])
    cols_d = dt_in("cols", [128, NCOL])
    convw_d = dt_in("convwT", [128, KC * 31])
    wsg_d = dt_in("wsgT", [128, 8 * 128])
    mask_d = dt_in("maskT", [128, 128])
    rb_d = dt_in("rb", [1, 36])
    bvecs_d = dt_in("bvecs", [8, D])
    wr_d = dt_in("wr", [D, 36])
    out = nc.dram_tensor("out", [T, D], F32, kind="ExternalOutput").ap()
    skind = "ExternalOutput" if debug else "Internal"
    yaT = nc.dram_tensor("yaT", [D, T], BF16, kind=skind).ap()
    x1s = nc.dram_tensor("x1s", [T, D], F32, kind=skind).ap()
    xpad = nc.dram_tensor("xpad", [NROWS, D], BF16, kind=skind).ap()
    ypad = nc.dram_tensor("ypad", [NROWS, D], F32, kind=skind).ap()
    if debug:
        dbg_dest = nc.dram_tensor("dbg_dest", [128, NGSB * 2], I32, kind="ExternalOutput").ap()
        dbg_ew = nc.dram_tensor("dbg_ew", [128, NGSB * 2], F32, kind="ExternalOutput").ap()

    R = Rec(nc)
    _rr = [0]

    def alt(*engs):
        _rr[0] += 1
        return engs[_rr[0] % len(engs)]

    def barrier():
        evs = {}
        for e in R.ENGS:
            if R.cnt[e] > 0:
                evs[id(R.sem[e])] = (R.sem[e], R.cnt[e])
        for k, (s, c) in R.dsem.items():
            if c > 0:
                evs[id(s)] = (s, c)
        for e in R.ENGS:
            waits = []
            for k, (s, v) in evs.items():
                if R.seen[e].get(k, 0) < v:
                    R.seen[e][k] = v
                    waits.append((s, v))
            R.prog[e].append((waits, None, None))

    PA = Alloc(nc)
    cols = PA.sb("cols", [128, NCOL], F32)
    colsh = PA.sb("colsh", [128, NCOL], F32)
    identf = PA.sb("identf", [128, 128], F32)
    identb = PA.sb("identb", [128, 128], BF16)
    ones_t = PA.sb("ones_t", [128, 512], BF16)
    onesf = PA.sb("onesf", [128, 128], F32)
    rbbc = PA.sb("rbbc", [128, 36], F32)
    negh = PA.sb("negh", [128, 256], F32)
    junk = [PA.sb("junk%d" % i, [128, D], BF16) for i in range(1)] * 2
    b_junk = [Buf("junk0")] * 2
    _jk = [0]
    iot = PA.sb("iot", [128, 128], I32)
    iotf = PA.sb("iotf", [128, 128], F32)
    DEST = PA.sb("DEST", [128, NGSB, 2], I32)
    GLM = PA.sb("GLM", [128, NGSB, 4], F32)
    DV = PA.sb("DV", [128, NGSB], F32)
    EW0 = PA.sb("EW0", [128, NGSB], F32)
    EW1 = PA.sb("EW1", [128, NGSB], F32)
    b_cols, b_colsh, b_identf, b_identb, b_ones, b_rows, b_negh, b_iot, b_Bbc = (Buf(n) for n in
        ["cols", "colsh", "identf", "identb", "ones", "rows", "negh", "iot", "Bbc"])
    b_DEST = [Buf("DEST%d" % i) for i in range(NGSB)]
    b_GLM = Buf("GLM"); b_DV = Buf("DV"); b_EW = Buf("EW")

    R.dma("sp", "c0", lambda h: h.dma_start(out=cols[:], in_=cols_d[:, :]), writes=[b_cols])
    R.dma("sp", "c1", lambda h: h.dma_start(out=rbbc[:], in_=rb_d[0:1, :].partition_broadcast(128)), writes=[b_rows])
    R.op("dve", lambda h: h.tensor_scalar(out=rbbc[:], in0=rbbc[:], scalar1=1.0 / 128, scalar2=None, op0=ALU.mult),
         reads=[b_rows], writes=[b_rows])
    R.op("pool", lambda h: h.iota(iot[:], pattern=[[1, 128]], base=0, channel_multiplier=-1), writes=[b_iot])
    R.op("dve", lambda h: h.tensor_copy(out=iotf[:], in_=iot[:]), reads=[b_iot], writes=[b_iot])
    R.op("dve", lambda h: h.tensor_scalar(out=identf[:], in0=iotf[:], scalar1=0.0, scalar2=None, op0=ALU.is_equal),
         reads=[b_iot], writes=[b_identf])
    R.op("dve", lambda h: h.tensor_copy(out=identb[:], in_=identf[:]), reads=[b_identf], writes=[b_identb])
    R.op("pool", lambda h: h.memset(ones_t[:], 1.0), writes=[b_ones])
    R.op("pool", lambda h: h.memset(onesf[:], 1.0), pwrites=[b_ones])
    R.op("pool", lambda h: h.memset(negh[:], -0.5), writes=[b_negh])
    R.op("dve", lambda h: h.tensor_scalar(out=colsh[:], in0=cols[:], scalar1=0.5, scalar2=None, op0=ALU.mult),
         reads=[b_cols], writes=[b_colsh])

    def col(name, j):
        return cols[:, CI[name] + j:CI[name] + j + 1]

    def colh(name, j):
        return colsh[:, CI[name] + j:CI[name] + j + 1]

    def mm_group(psum_ap, psum_buf, terms, partial=None):
        n = len(terms)
        first, last = (True, True) if partial is None else partial
        fns = []
        per = []
        for i, (l, r, bs) in enumerate(terms):
            fns.append(lambda h, l=l, r=r, st=(first and i == 0), sp=(last and i == n - 1):
                       h.matmul(psum_ap, lhsT=l, rhs=r, start=st, stop=sp))
            per.append(list(bs))
        if first:
            R.group("pe", fns, writes=[psum_buf], per_reads=per)
        else:
            R.group("pe", fns, pwrites=[psum_buf], per_reads=per)

    def rms_head(xs_ap, b_xs, ss_ap, rs_ap, b_st):
        _jk[0] ^= 1
        jk = _jk[0]
        R.op("act", lambda h: h.activation(out=junk[jk][:], in_=xs_ap, func=AF.Square, accum_out=ss_ap),
             reads=[b_xs], writes=[b_st, b_junk[jk]])
        R.op("dve", lambda h: h.tensor_scalar(out=ss_ap, in0=ss_ap, scalar1=1.0 / D, scalar2=EPS,
                                              op0=ALU.mult, op1=ALU.add), reads=[b_st], writes=[b_st])
        R.op("pool", lambda h: h.tensor_tensor(out=rs_ap, in0=ss_ap, in1=negh[:, 0:1], op=ALU.pow),
             reads=[b_st, b_negh], writes=[b_st])

    def cast_op(eng, out_ap, in_ap, scale, reads, pwrites):
        if eng == "act":
            if scale is None:
                R.op("act", lambda h: h.activation(out=out_ap, in_=in_ap, func=AF.Copy), reads=reads, pwrites=pwrites)
            else:
                R.op("act", lambda h: h.activation(out=out_ap, in_=in_ap, func=AF.Identity, scale=scale), reads=reads, pwrites=pwrites)
        else:
            if scale is None:
                R.op("dve", lambda h: h.tensor_copy(out=out_ap, in_=in_ap), reads=reads, pwrites=pwrites)
            else:
                R.op("dve", lambda h: h.tensor_scalar(out=out_ap, in0=in_ap, scalar1=scale, scalar2=None, op0=ALU.mult),
                     reads=reads, pwrites=pwrites)

    A1 = Alloc(nc)
    Bbc = A1.sb("Bbc1", [128, 2, D], BF16)
    Wa = A1.sb("Wa", [128, KC, 3072], BF16)
    Wco = A1.sb("Wco", [128, KC, D], BF16)
    Dg = A1.sb("Dg", [128, KC, 31, 128], BF16)
    cw = A1.sb("cw", [128, KC * 31], F32)
    onesM = A1.sb("onesM", [128, 128], BF16)
    A1s = Alloc(nc)
    stage = [A1s.sb("stage%d" % i, [128, 2048], F32) for i in range(2)]
    b_Wa = [Buf("Wa%d" % k) for k in range(KC)]
    b_Wco = [Buf("Wco%d" % k) for k in range(KC)]
    b_Dg = [Buf("Dg%d" % j) for j in range(KC)]
    b_cw = Buf("cw"); b_onesM = Buf("onesM")
    b_stage = [Buf("stage0"), Buf("stage1")]
    b_xs = [Buf("xs%d" % i) for i in range(2)]
    b_hb = [Buf("hb%d" % i) for i in range(2)]
    b_st1 = [Buf("st1_%d" % i) for i in range(2)]
    b_hT = [[Buf("hT%d_%d" % (i, s)) for s in range(NSB)] for i in range(2)]
    b_Gm = [Buf("Gm%d" % j) for j in range(KC)]
    b_Gh = [Buf("Gh%d" % j) for j in range(KC)]
    b_th = [Buf("th0"), Buf("th1")]
    b_Y = [Buf("Y%d" % j) for j in range(KC)]
    b_Yb = [Buf("Yb%d" % j) for j in range(KC)]
    b_Ysq = [Buf("Ysq0"), Buf("Ysq1")]
    b_lnr = Buf("lnr"); b_lnv = Buf("lnv"); b_Dr = Buf("Dr")
    b_dn = [Buf("dn0"), Buf("dn1")]
    b_dn2 = [Buf("dn2_0"), Buf("dn2_1")]
    b_A = [Buf("A%d" % j) for j in range(KC)]
    b_tga = [Buf("tga%d" % j) for j in range(KC)]
    b_ost = [[Buf("ost%d_%d" % (i, j)) for j in range(KC)] for i in range(1)]
    b_tr = [Buf("tr0"), Buf("tr1")]
    b_mmb = [Buf("mmb%d" % i) for i in range(4)]
    b_stM = Buf("stM"); b_stQ = Buf("stQ")
    b_yaT = [Buf("yaT%d" % t) for t in range(NT)]
    _mm = [0]

    def next_bank():
        _mm[0] = (_mm[0] + 1) % len(mmb)
        return mmb[_mm[0]], b_mmb[_mm[0]]

    _st = [0]

    def load_cast(dram_ap, ncols, out_ap, out_buf, scale_ap=None, extra_reads=()):
        _st[0] ^= 1
        sl = _st[0]
        R.dma("sp", "stg%d" % sl, lambda h: h.dma_start(out=stage[sl][:, 0:ncols], in_=dram_ap), writes=[b_stage[sl]])
        cast_op(alt("dve", "act"), out_ap, stage[sl][:, 0:ncols], scale_ap, [b_stage[sl], b_cols], [out_buf])

    R.dma("sp", "c2", lambda h: h.dma_start(out=cw[:], in_=convw_d[:, :]), writes=[b_cw])
    R.op("pool", lambda h: h.memset(onesM[:], 1.0 / D), writes=[b_onesM])
    for k in range(KC):
        rs = slice(k * 128, (k + 1) * 128)
        load_cast(w_in[rs, 0:2048], 2048, Wa[:, k, 0:2048], b_Wa[k], col("g1", k))
        load_cast(w_in[rs, 4096:5120], 1024, Wa[:, k, 2048:3072], b_Wa[k], col("g1", k))
        load_cast(w_co[rs, :], 1024, Wco[:, k, :], b_Wco[k])
    R.op("dve", lambda h: h.tensor_scalar(out=cw[:], in0=cw[:], scalar1=0.5, scalar2=None, op0=ALU.mult),
         reads=[b_cw], writes=[b_cw])
    for j in range(KC):
        for tap in range(31):
            cast_op(alt("dve", "act"), Dg[:, j, tap, :], identf[:], cw[:, j * 31 + tap:j * 31 + tap + 1],
                    [b_identf, b_cw], [b_Dg[j]])
    for i, nm in enumerate(["b_a1", "b_co"]):
        _st[0] ^= 1
        sl = _st[0]
        R.dma("sp", "stg%d" % sl, lambda h, sl=sl, nm=nm: h.dma_start(out=stage[sl][:, 0:D],
              in_=bvecs_d[BV[nm]:BV[nm] + 1, :].partition_broadcast(128)), writes=[b_stage[sl]])
        R.op("dve", lambda h, sl=sl, i=i: h.tensor_scalar(out=Bbc[:, i, :], in0=stage[sl][:, 0:D], scalar1=1.0 / 128, scalar2=None,
                                                         op0=ALU.mult), reads=[b_stage[sl]], pwrites=[b_Bbc])
    barrier()
    A1s.free()
    xs = [A1.sb("xs%d" % i, [128, D], F32) for i in range(2)]
    hb = [A1.sb("hb%d" % i, [128, D], BF16) for i in range(2)]
    st1 = [A1.sb("st1_%d" % i, [128, 2], F32) for i in range(2)]
    hT = [A1.sb("hT%d" % i, [128, KC, TT], BF16) for i in range(2)]
    G = A1.sb("G", [128, KC, 32 + TT], BF16)
    th = [A1.sb("th%d" % i, [128, TT], F32) for i in range(2)]
    Y = A1.sb("Y", [128, KC, TT], F32)
    Yb = A1.sb("Yb", [128, KC, TT], BF16)
    Ysq = [A1.sb("Ysq%d" % i, [128, TT], BF16) for i in range(2)]
    lnr = A1.sb("lnr", [128, 2, 2], F32)
    lnv = A1.sb("lnv", [128, 4, 2], F32)
    Dr = A1.sb("Dr", [128, 2, 2, 128], F32)
    dn = [A1.sb("dn%d" % i, [128, TT], F32) for i in range(2)]
    dn2 = [A1.sb("dn2_%d" % i, [128, TT], F32) for i in range(2)]
    Aact = A1.sb("Aact", [128, KC, TT], BF16)
    tga = A1.sb("tga", [128, KC, TT], BF16)
    ostage = [A1.sb("ostage%d" % i, [128, KC, TT], BF16) for i in range(1)]
    tr = [A1.ps("tr%d" % i, [128, KC, 128], BF16) for i in range(2)]
    mmb = [A1.ps("mmb%d" % i, [128, 512], F32) for i in range(4)]
    stM = A1.ps("stM", [128, 512], F32)
    stQ = A1.ps("stQ", [128, 2, 2, 128], F32)
    R.op("pool", lambda h: h.memset(G[:, :, 0:32], 0.0), writes=b_Gh)

    def p1_head_a(t):
        for sbi in range(NSB):
            xsl = sbi
            tok0 = t * TT + sbi * 128
            R.dma("sp", "xs%d" % xsl, lambda h, xsl=xsl, tok0=tok0: h.dma_start(out=xs[xsl][:], in_=x[tok0:tok0 + 128, :]),
                  writes=[b_xs[xsl]])
            rms_head(xs[xsl][:], b_xs[xsl], st1[xsl][:, 0:1], st1[xsl][:, 1:2], b_st1[xsl])
            R.op("dve", lambda h, xsl=xsl, sbi=sbi: h.tensor_scalar(out=hb[sbi][:], in0=xs[xsl][:], scalar1=st1[xsl][:, 1:2],
                                                                     scalar2=None, op0=ALU.mult),
                 reads=[b_xs[xsl], b_st1[xsl]], writes=[b_hb[sbi]])

    def p1_head_b(t):
        ts = t % 2
        for sbi in range(NSB):
            R.group("pe", [(lambda h, k=k, sbi=sbi: h.transpose(out=tr[sbi][:, k, :], in_=hb[sbi][:, k * 128:(k + 1) * 128],
                                                               identity=identb[:])) for k in range(KC)],
                    reads=[b_hb[sbi], b_identb], writes=[b_tr[sbi]])
            R.op("act", lambda h, sbi=sbi, ts=ts: h.activation(out=hT[ts][:, :, sbi * 128:(sbi + 1) * 128], in_=tr[sbi][:],
                                                               func=AF.Copy),
                 reads=[b_tr[sbi]], writes=[b_hT[ts][sbi]])

    def p1_A(t, j):
        ts = t % 2
        hTr = b_hT[ts]
        p1, bp1 = next_bank()
        terms = [(Bbc[:, BI["b_a1"], j * 128:(j + 1) * 128], ones_t[:, 0:TT], [b_Bbc, b_ones])]
        terms += [(Wa[:, k, j * 128:(j + 1) * 128], hT[ts][:, k, :], [b_Wa[k]] + hTr) for k in range(KC)]
        mm_group(p1[:, 0:TT], bp1, terms)
        p2, bp2 = next_bank()
        terms = [(Wa[:, k, D + j * 128:D + (j + 1) * 128], hT[ts][:, k, :], [b_Wa[k]] + hTr) for k in range(KC)]
        mm_group(p2[:, 0:TT], bp2, terms)
        tsl = j % 2
        R.op("act", lambda h: h.activation(out=th[tsl][:], in_=p2[:, 0:TT], func=AF.Tanh, bias=colh("b_a2", j), scale=0.5),
             reads=[bp2, b_colsh], writes=[b_th[tsl]])
        R.op("dve", lambda h: h.scalar_tensor_tensor(out=G[:, j, 32:32 + TT], in0=th[tsl][:], scalar=1.0, in1=p1[:, 0:TT],
                                                     op0=ALU.add, op1=ALU.mult),
             reads=[b_th[tsl], bp1], writes=[b_Gm[j]])

    def p1_C(t, j, last_in_seq):
        pc, bpc = next_bank()
        terms = [(Dg[:, j, tap, :], G[:, j, 2 + tap:2 + tap + TT], [b_Dg[j], b_Gm[j], b_Gh[j]]) for tap in range(31)]
        mm_group(pc[:, 0:TT], bpc, terms)
        R.op("act", lambda h: h.activation(out=Y[:, j, :], in_=pc[:, 0:TT], func=AF.Identity, bias=col("conv_b", j), scale=1.0),
             reads=[bpc, b_cols], writes=[b_Y[j]])
        if last_in_seq:
            R.op("pool", lambda h: h.memset(G[:, j, 0:32], 0.0), writes=[b_Gh[j]])
        else:
            R.op("pool", lambda h: h.tensor_copy(out=G[:, j, 0:32], in_=G[:, j, TT:TT + 32]), reads=[b_Gm[j]], writes=[b_Gh[j]])
        R.op("dve", lambda h: h.tensor_copy(out=Yb[:, j, :], in_=Y[:, j, :]), reads=[b_Y[j]], writes=[b_Yb[j]])
        qs = j % 2
        R.op("act", lambda h: h.activation(out=Ysq[qs][:], in_=Y[:, j, :], func=AF.Square), reads=[b_Y[j]], writes=[b_Ysq[qs]])

    def p1_S(t, j):
        qs = j % 2
        fns = []
        for sb in range(NSB):
            c0 = (sb * 2 + 0) * 8 + j
            c1 = (sb * 2 + 1) * 8 + j
            fns.append(lambda h, sb=sb, c0=c0: h.matmul(stM[:, c0:c0 + 1], lhsT=Yb[:, j, sb * 128:(sb + 1) * 128], rhs=onesM[:, 0:1],
                                                        start=True, stop=True))
            fns.append(lambda h, sb=sb, c1=c1: h.matmul(stM[:, c1:c1 + 1], lhsT=Ysq[qs][:, sb * 128:(sb + 1) * 128], rhs=onesM[:, 0:1],
                                                        start=True, stop=True))
        if j == 0:
            R.group("pe", fns, reads=[b_onesM, b_Yb[j], b_Ysq[qs]], writes=[b_stM])
        else:
            R.group("pe", fns, reads=[b_onesM, b_Yb[j], b_Ysq[qs]], pwrites=[b_stM])

    def p1_gates(t, jos=None):
        ts = t % 2
        for jo in (range(KC) if jos is None else jos):
            pg, bpg = next_bank()
            terms = [(Wa[:, k, 2048 + jo * 128:2048 + (jo + 1) * 128], hT[ts][:, k, :], [b_Wa[k]] + b_hT[ts]) for k in range(KC)]
            mm_group(pg[:, 0:TT], bpg, terms)
            R.op("act", lambda h, jo=jo, pg=pg: h.activation(out=tga[:, jo, :], in_=pg[:, 0:TT], func=AF.Tanh,
                                                             bias=colh("b_ga", jo), scale=0.5),
                 reads=[bpg, b_colsh], writes=[b_tga[jo]])

    def p1_tail(t):
        os_ = 0
        R.op("dve", lambda h: h.tensor_reduce(out=lnr[:], in_=stM[:, 0:32].rearrange("p (a j) -> p a j", j=8), axis=AX.X, op=ALU.add),
             reads=[b_stM], writes=[b_lnr])
        R.op("dve", lambda h: h.tensor_tensor(out=lnv[:, 0, :], in0=lnr[:, :, 0], in1=lnr[:, :, 0], op=ALU.mult),
             reads=[b_lnr], writes=[b_lnv])
        R.op("dve", lambda h: h.scalar_tensor_tensor(out=lnv[:, 1, :], in0=lnr[:, :, 1], scalar=EPS, in1=lnv[:, 0, :],
                                                     op0=ALU.add, op1=ALU.subtract), reads=[b_lnr, b_lnv], writes=[b_lnv])
        R.op("pool", lambda h: h.tensor_tensor(out=lnv[:, 2, :], in0=lnv[:, 1, :], in1=negh[:, 0:2], op=ALU.pow),
             reads=[b_lnv, b_negh], writes=[b_lnv])
        R.op("dve", lambda h: h.scalar_tensor_tensor(out=lnv[:, 3, :], in0=lnr[:, :, 0], scalar=-1.0, in1=lnv[:, 2, :],
                                                     op0=ALU.mult, op1=ALU.mult), reads=[b_lnr, b_lnv], writes=[b_lnv])
        for sb in range(NSB):
            for st_ in range(2):
                R.op("dve", lambda h, sb=sb, st_=st_: h.tensor_scalar(out=Dr[:, sb, st_, :], in0=identf[:],
                                                                      scalar1=lnv[:, 2 + st_, sb:sb + 1], scalar2=None, op0=ALU.mult),
                     reads=[b_identf, b_lnv], pwrites=[b_Dr])

    def p1_tail_mm(t):
        R.group("pe", [(lambda h, sb=sb: h.matmul(stQ[:, sb, :, :], lhsT=onesf[:], rhs=Dr[:, sb, :, :], start=True, stop=True))
                       for sb in range(NSB)], reads=[b_ones, b_Dr], writes=[b_stQ])

    def p1_tail_c(t, js=None):
        for j in (range(KC) if js is None else js):
            s2 = j % 2
            R.op("dve", lambda h, j=j, s2=s2: h.tensor_tensor(out=dn[s2][:].rearrange("p (a t) -> p a t", a=2),
                                                              in0=Y[:, j, :].rearrange("p (a t) -> p a t", a=2),
                                                              in1=stQ[:, :, 0, :], op=ALU.mult),
                 reads=[b_Y[j], b_stQ], writes=[b_dn[s2]])
            R.op("dve", lambda h, s2=s2: h.tensor_tensor(out=dn2[s2][:].rearrange("p (a t) -> p a t", a=2),
                                                         in0=dn[s2][:].rearrange("p (a t) -> p a t", a=2),
                                                         in1=stQ[:, :, 1, :], op=ALU.add),
                 reads=[b_dn[s2], b_stQ], writes=[b_dn2[s2]])
            R.op("act", lambda h, j=j, s2=s2: h.activation(out=Aact[:, j, :], in_=dn2[s2][:], func=AF.Silu,
                                                           scale=col("ln_g", j), bias=col("ln_b", j)),
                 reads=[b_dn2[s2], b_cols], writes=[b_A[j]])

    def p1_po(t):
        os_ = 0
        for jo in range(KC):
            po, bpo = next_bank()
            terms = [(Bbc[:, BI["b_co"], jo * 128:(jo + 1) * 128], ones_t[:, 0:TT], [b_Bbc, b_ones])]
            terms += [(Wco[:, k, jo * 128:(jo + 1) * 128], Aact[:, k, :], [b_Wco[k], b_A[k]]) for k in range(KC)]
            mm_group(po[:, 0:TT], bpo, terms)
            R.op("dve", lambda h, jo=jo, po=po: h.scalar_tensor_tensor(out=ostage[os_][:, jo, :], in0=tga[:, jo, :], scalar=1.0,
                                                                        in1=po[:, 0:TT], op0=ALU.add, op1=ALU.mult),
                 reads=[b_tga[jo], bpo], writes=[b_ost[os_][jo]])
        R.dma("sp", "ost%d" % os_, lambda h: h.dma_start(
            out=yaT.rearrange("(j p) t -> p j t", p=128)[:, :, t * TT:(t + 1) * TT], in_=ostage[os_][:]),
            reads=b_ost[os_], writes=[b_yaT[t]])

    def p1_body(t, i0, i1):
        last_in_seq = (t % TPS == TPS - 1)
        for i in range(i0, i1):
            if i < KC:
                p1_A(t, i)
            if 1 <= i <= KC:
                p1_C(t, i - 1, last_in_seq)
            if i >= 2:
                p1_S(t, i - 2)

    p1_head_a(0)
    p1_head_b(0)
    p1_body(0, 0, 2)
    for t in range(NT):
        nxt = t + 1 < NT
        if nxt:
            p1_head_a(t + 1)
        p1_body(t, 2, KC + 2)
        p1_tail(t)
        if nxt:
            p1_head_b(t + 1)
        p1_tail_mm(t)
        for j in range(KC):
            p1_tail_c(t, [j])
            p1_gates(t, [j])
        p1_po(t)
        if nxt:
            p1_body(t + 1, 0, 2)
    barrier()
    A1.free()

    A2 = Alloc(nc)
    Bbc2 = A2.sb("Bbc2", [128, 2, D], BF16)
    Wz = A2.sb("Wz", [128, KC, 2048], BF16)
    Wgb = A2.sb("Wgb", [128, KC, D], BF16)
    Wsgo = A2.sb("Wsgo", [128, KC, D], BF16)
    Wout = A2.sb("Wout", [128, KC, D], BF16)
    WmT = A2.sb("WmT", [128, 8, 128], BF16)
    Cst = A2.sb("Cst", [128, 8, 128], F32)
    Gbc = A2.sb("Gbc", [128, D], F32)
    Boutbc = A2.sb("Boutbc", [128, D], F32)
    Wr = A2.sb("Wr", [128, KC, 36], F32)
    iox = A2.sb("iox", [128, 32], I32)
    iotx32 = A2.sb("iotx32", [128, 32], F32)
    iotg32 = A2.sb("iotg32", [128, 32], F32)
    Ustrict = A2.sb("Ustrict", [128, 128], BF16)
    Acum = [A2.sb("Acum%d" % i, [128, 32], BF16) for i in range(2)]
    b_Wz = [Buf("Wz%d" % k) for k in range(KC)]
    b_Wgb = [Buf("Wgb%d" % k) for k in range(KC)]
    b_Wsgo = [Buf("Wsgo%d" % k) for k in range(KC)]
    b_Wout = [Buf("Wout%d" % k) for k in range(KC)]
    b_WmT = Buf("WmT"); b_Cst = Buf("Cst"); b_Gbc = Buf("Gbc"); b_Bout = Buf("Bout"); b_Wr = Buf("Wr")
    b_rc = Buf("routeconst"); b_Acum = [Buf("Acum0"), Buf("Acum1")]

    A2s = Alloc(nc)
    stageB = [A2s.sb("stage2_%d" % i, [128, 2048], F32) for i in range(2)]
    b_stage = [Buf("stage0"), Buf("stage1")]
    wsg_f = A2s.sb("wsg_f", [128, 8 * 128], F32)
    maskf = A2s.sb("maskf", [128, 128], F32)
    bsbc = A2s.sb("bsbc", [128, D], F32)
    wr_f = A2s.sb("wr_f", [128, KC, 36], F32)
    rwp = A2s.ps("rwp", [128, 512], F32)
    b_wsgf = Buf("wsgf"); b_maskf = Buf("maskf"); b_bsbc = Buf("bsbc"); b_wrf = Buf("wrf"); b_rwp = Buf("rwp")

    def load_cast2(dram_ap, ncols, out_ap, out_buf, scale=None):
        _st[0] ^= 1
        sl = _st[0]
        R.dma("sp", "stg%d" % sl, lambda h: h.dma_start(out=stageB[sl][:, 0:ncols], in_=dram_ap), writes=[b_stage[sl]])
        cast_op(alt("dve", "act"), out_ap, stageB[sl][:, 0:ncols], scale, [b_stage[sl], b_cols], [out_buf])

    for k in range(KC):
        rs = slice(k * 128, (k + 1) * 128)
        load_cast2(w_in[rs, 2048:4096], 2048, Wz[:, k, :], b_Wz[k], col("g1", k))
        load_cast2(w_in[rs, 5120:6144], 1024, Wgb[:, k, :], b_Wgb[k], col("g1", k))
        load_cast2(w_sgo[rs, :], 1024, Wsgo[:, k, :], b_Wsgo[k])
        load_cast2(w_out[rs, :], 1024, Wout[:, k, :], b_Wout[k], 0.5)
    for i, nm in enumerate(["b_sgo", "b_v"]):
        _st[0] ^= 1
        sl = _st[0]
        R.dma("sp", "stg%d" % sl, lambda h, sl=sl, nm=nm: h.dma_start(out=stageB[sl][:, 0:D],
              in_=bvecs_d[BV[nm]:BV[nm] + 1, :].partition_broadcast(128)), writes=[b_stage[sl]])
        R.op("dve", lambda h, sl=sl, i=i: h.tensor_scalar(out=Bbc2[:, i, :], in0=stageB[sl][:, 0:D], scalar1=1.0 / 128, scalar2=None,
                                                         op0=ALU.mult), reads=[b_stage[sl]], pwrites=[b_Bbc])
    R.dma("sp", "c3", lambda h: h.dma_start(out=wsg_f[:], in_=wsg_d[:, :]), writes=[b_wsgf])
    R.dma("sp", "c4", lambda h: h.dma_start(out=maskf[:], in_=mask_d[:, :]), writes=[b_maskf])
    R.dma("sp", "c5", lambda h: h.dma_start(out=Gbc[:], in_=bvecs_d[BV["sg_g"]:BV["sg_g"] + 1, :].partition_broadcast(128)), writes=[b_Gbc])
    R.dma("sp", "c6", lambda h: h.dma_start(out=Boutbc[:], in_=bvecs_d[BV["b_out"]:BV["b_out"] + 1, :].partition_broadcast(128)), writes=[b_Bout])
    R.dma("sp", "c7", lambda h: h.dma_start(out=bsbc[:], in_=bvecs_d[BV["b_sg"]:BV["b_sg"] + 1, :].partition_broadcast(128)), writes=[b_bsbc])
    R.dma("sp", "c8", lambda h: h.dma_start(out=wr_f[:], in_=wr_d.rearrange("(k p) n -> p k n", p=128)), writes=[b_wrf])
    R.op("dve", lambda h: h.tensor_scalar(out=Ustrict[:], in0=iotf[:], scalar1=0.0, scalar2=None, op0=ALU.is_gt),
         reads=[b_iot], writes=[b_rc])
    R.op("pool", lambda h: h.iota(iox[:], pattern=[[1, 32]], base=0, channel_multiplier=0), pwrites=[b_rc])
    R.op("dve", lambda h: h.tensor_copy(out=iotx32[:], in_=iox[:]), reads=[b_rc], pwrites=[b_rc])
    R.op("pool", lambda h: h.iota(iox[:], pattern=[[1, 4], [0, 8]], base=0, channel_multiplier=0), reads=[b_rc], pwrites=[b_rc])
    R.op("dve", lambda h: h.tensor_copy(out=iotg32[:], in_=iox[:]), reads=[b_rc], pwrites=[b_rc])
    R.op("pool", lambda h: h.memset(Acum[0][:], 0.0), writes=[b_Acum[0]])
    for k in range(KC):
        R.op("dve", lambda h, k=k: h.tensor_scalar(out=Wr[:, k, :], in0=wr_f[:, k, :], scalar1=col("g2", k), scalar2=None,
                                                   op0=ALU.mult), reads=[b_wrf, b_cols], pwrites=[b_Wr])
    for hd in range(8):
        R.op("dve", lambda h, hd=hd: h.tensor_tensor(out=WmT[:, hd, :], in0=wsg_f[:, hd * 128:(hd + 1) * 128], in1=maskf[:],
                                                     op=ALU.mult), reads=[b_wsgf, b_maskf], pwrites=[b_WmT])
    for hd in range(8):
        R.group("pe", [lambda h, hd=hd: h.matmul(rwp[:, 0:128], lhsT=ones_t[:, 0:128], rhs=WmT[:, hd, :], start=True, stop=True)],
                reads=[b_ones, b_WmT], writes=[b_rwp])
        R.op("dve", lambda h, hd=hd: h.scalar_tensor_tensor(out=Cst[:, hd, :], in0=rwp[:, 0:128], scalar=col("sg_b", hd),
                                                            in1=bsbc[:, hd * 128:(hd + 1) * 128], op0=ALU.mult, op1=ALU.add),
             reads=[b_rwp, b_cols, b_bsbc], pwrites=[b_Cst])
    barrier()
    A2s.free()

    xs2 = [A2.sb("xs2_%d" % i, [128, D], F32) for i in range(6)]
    hbB = [A2.sb("hb2_%d" % i, [128, D], BF16) for i in range(2)]
    st2 = [A2.sb("st2_%d" % i, [128, 4], F32) for i in range(6)]
    hTB = [A2.sb("hT2_%d" % i, [128, KC, TT], BF16) for i in range(1)] * 2
    uT = A2.sb("uT", [128, KC, TT], BF16)
    v = [A2.sb("v%d" % i, [128, D], F32) for i in range(2)]
    bst = [A2.sb("bst%d" % i, [128, 2, 6], F32) for i in range(2)]
    mv = [A2.sb("mv%d" % i, [128, 4], F32) for i in range(2)]
    vn = [A2.sb("vn%d" % i, [128, D], BF16) for i in range(2)]
    tmpS = [A2.sb("tmpS%d" % i, [128, 8, 128], F32) for i in range(1)] * 2
    sT = A2.sb("sT", [128, KC, TT], BF16)
    tgb = A2.sb("tgb", [128, KC, TT], BF16)
    gaya = [A2.sb("gaya%d" % i, [128, KC, TT], BF16) for i in range(1)] * 2
    t1 = [A2.sb("t1_%d" % i, [128, TT], F32) for i in range(2)]
    mixT = A2.sb("mixT", [128, KC, TT], BF16)
    h2f = [A2.sb("h2f%d" % i, [128, D], F32) for i in range(2)]
    h2b = [A2.sb("h2b%d" % i, [128, D], BF16) for i in range(4)]
    h2T = [A2.sb("h2T%d" % i, [128, KC, 128], F32) for i in range(1)] * 2
    rt = []
    for i in range(4):
        d = {}
        for nm, shp, dty in [("L", [128, 36], F32), ("gl8", [128, 8], F32), ("t8g", [128, 8], F32), ("i8g", [128, 8], U32),
                             ("gself", [128, 1], F32), ("pen32", [128, 32], F32), ("M32", [128, 32], F32),
                             ("t8e", [128, 8], F32), ("i8e", [128, 8], U32), ("ef", [128, 2], F32),
                             ("A0", [128, 32], F32), ("A1", [128, 32], F32), ("Ab", [128, 32], BF16),
                             ("prod", [128, 32], F32), ("pos", [128, 2], F32), ("dest", [128, 2], F32)]:
            d[nm] = A2.sb("rt%d_%s" % (i, nm), shp, dty)
            d["b_" + nm] = Buf("rt%d_%s" % (i, nm))
        rt.append(d)
    tr2 = A2.ps("tr2", [128, KC, 128], BF16)
    big = [A2.ps("big%d" % i, [128, KC, 128], F32) for i in range(2)]
    mmbB = [A2.ps("mmb2_%d" % i, [128, 512], F32) for i in range(3)]
    b_xs2 = [Buf("xs2_%d" % i) for i in range(6)]
    b_hb = [Buf("hb0"), Buf("hb1")]
    b_st2 = [Buf("st2_%d" % i) for i in range(6)]
    b_hT = [[Buf("hT2_%d_%d" % (i, s)) for s in range(NSB)] for i in range(1)] * 2
    b_uT = [Buf("uT%d" % j) for j in range(KC)]
    b_v = [[Buf("v%d_%d" % (i, n)) for n in range(2)] for i in range(2)]
    b_bst = [Buf("bst0"), Buf("bst1")]
    b_mv = [Buf("mv0"), Buf("mv1")]
    b_vn = [Buf("vn0"), Buf("vn1")]
    b_tmpS = [Buf("tmpS0")] * 2
    b_sT = [Buf("sT%d" % s) for s in range(NSB)]
    b_tgb = [Buf("tgb%d" % j) for j in range(KC)]
    b_gaya = [Buf("gaya0")] * 2
    b_t1 = [Buf("t1_0"), Buf("t1_1")]
    b_mixT = [Buf("mixT%d" % j) for j in range(KC)]
    b_h2f = [Buf("h2f0"), Buf("h2f1")]
    b_h2b = [Buf("h2b%d" % i) for i in range(4)]
    b_h2T = [Buf("h2T0")] * 2
    b_tr2 = Buf("tr2"); b_big = [Buf("big0"), Buf("big1")]
    b_mmb = [Buf("mmb2_%d" % i) for i in range(3)]
    b_x1s = [Buf("x1s%d" % i) for i in range(NGSB)]
    b_xpad = Buf("xpad")
    _mm[0] = 0
    _bg = [0]

    def next_bank2():
        _mm[0] = (_mm[0] + 1) % len(mmbB)
        return mmbB[_mm[0]], b_mmb[_mm[0]]

    def next_big():
        _bg[0] ^= 1
        return big[_bg[0]], b_big[_bg[0]]

    for i in range(4):
        R.op("pool", lambda h, i=i: h.memset(rt[i]["gl8"][:], -1e30), writes=[rt[i]["b_gl8"]])

    def p2_gaya(t):
        ts = t % 2
        R.dma("sp", "gaya%d" % ts, lambda h: h.dma_start(
            out=gaya[ts][:], in_=yaT.rearrange("(j p) t -> p j t", p=128)[:, :, t * TT:(t + 1) * TT]),
            reads=[b_yaT[t]], writes=[b_gaya[ts]])

    def p2_head_a(t):
        for sbi in range(NSB):
            xsl = (t % 3) * NSB + sbi
            tok0 = t * TT + sbi * 128
            R.dma("sp", "xs%d" % xsl, lambda h, xsl=xsl, tok0=tok0: h.dma_start(out=xs2[xsl][:], in_=x[tok0:tok0 + 128, :]),
                  writes=[b_xs2[xsl]])
            rms_head(xs2[xsl][:], b_xs2[xsl], st2[xsl][:, 0:1], st2[xsl][:, 1:2], b_st2[xsl])
            R.op("dve", lambda h, xsl=xsl, sbi=sbi: h.tensor_scalar(out=hbB[sbi][:], in0=xs2[xsl][:], scalar1=st2[xsl][:, 1:2],
                                                                     scalar2=None, op0=ALU.mult),
                 reads=[b_xs2[xsl], b_st2[xsl]], writes=[b_hb[sbi]])
            R.op("dve", lambda h, xsl=xsl: h.tensor_tensor(out=xs2[xsl][:], in0=xs2[xsl][:], in1=Boutbc[:], op=ALU.add),
                 reads=[b_Bout], writes=[b_xs2[xsl]])

    def p2_head_b(t):
        ts = t % 2
        for sbi in range(NSB):
            R.group("pe", [(lambda h, k=k, sbi=sbi: h.transpose(out=tr2[:, k, :], in_=hbB[sbi][:, k * 128:(k + 1) * 128],
                                                               identity=identb[:])) for k in range(KC)],
                    reads=[b_hb[sbi], b_identb], writes=[b_tr2])
            R.op("act", lambda h, sbi=sbi, ts=ts: h.activation(out=hTB[ts][:, :, sbi * 128:(sbi + 1) * 128], in_=tr2[:],
                                                               func=AF.Copy),
                 reads=[b_tr2], writes=[b_hT[ts][sbi]])

    def p2_V(t):
        ts = t % 2
        for sbi in range(NSB):
            for n in range(2):
                pv, bpv = next_bank2()
                terms = [(ones_t[:, 0:128], Bbc2[:, BI["b_v"], n * 512:(n + 1) * 512], [b_Bbc, b_ones])]
                terms += [(hTB[ts][:, k, sbi * 128:(sbi + 1) * 128], Wz[:, k, D + n * 512:D + (n + 1) * 512],
                           [b_hT[ts][sbi], b_Wz[k]]) for k in range(KC)]
                mm_group(pv[:, 0:512], bpv, terms)
                R.op("act", lambda h, sbi=sbi, n=n, pv=pv: h.activation(out=v[sbi][:, n * 512:(n + 1) * 512], in_=pv[:, 0:512],
                                                                        func=AF.Gelu),
                     reads=[bpv], writes=[b_v[sbi][n]])

    def p2_U(t):
        ts = t % 2
        for j in range(KC):
            pu, bpu = next_bank2()
            terms = [(Wz[:, k, j * 128:(j + 1) * 128], hTB[ts][:, k, :], [b_Wz[k]] + b_hT[ts]) for k in range(KC)]
            mm_group(pu[:, 0:TT], bpu, terms)
            R.op("act", lambda h, j=j, pu=pu: h.activation(out=uT[:, j, :], in_=pu[:, 0:TT], func=AF.Gelu,
                                                           bias=col("b_u", j), scale=1.0),
                 reads=[bpu, b_cols], writes=[b_uT[j]])

    def p2_GB(t):
        ts = t % 2
        for jo in range(KC):
            pg, bpg = next_bank2()
            terms = [(Wgb[:, k, jo * 128:(jo + 1) * 128], hTB[ts][:, k, :], [b_Wgb[k]] + b_hT[ts]) for k in range(KC)]
            mm_group(pg[:, 0:TT], bpg, terms)
            R.op("act", lambda h, jo=jo, pg=pg: h.activation(out=tgb[:, jo, :], in_=pg[:, 0:TT], func=AF.Tanh,
                                                             bias=colh("b_gb", jo), scale=0.5),
                 reads=[bpg, b_colsh], writes=[b_tgb[jo]])

    def p2_LN(t):
        for sbi in range(NSB):
            for n in range(2):
                R.op("dve", lambda h, sbi=sbi, n=n: h.bn_stats(out=bst[sbi][:, n, :], in_=v[sbi][:, n * 512:(n + 1) * 512]),
                     reads=[b_v[sbi][n]], pwrites=[b_bst[sbi]])
            R.op("dve", lambda h, sbi=sbi: h.bn_aggr(out=mv[sbi][:, 0:2], in_=bst[sbi][:]), reads=[b_bst[sbi]], writes=[b_mv[sbi]])
            R.op("dve", lambda h, sbi=sbi: h.tensor_scalar(out=mv[sbi][:, 2:3], in0=mv[sbi][:, 1:2], scalar1=EPS, scalar2=None,
                                                           op0=ALU.add), reads=[b_mv[sbi]], writes=[b_mv[sbi]])
            R.op("pool", lambda h, sbi=sbi: h.tensor_tensor(out=mv[sbi][:, 3:4], in0=mv[sbi][:, 2:3], in1=negh[:, 0:1], op=ALU.pow),
                 reads=[b_mv[sbi], b_negh], writes=[b_mv[sbi]])
            R.op("dve", lambda h, sbi=sbi: h.tensor_scalar(out=v[sbi][:], in0=v[sbi][:], scalar1=mv[sbi][:, 0:1],
                                                           scalar2=mv[sbi][:, 3:4], op0=ALU.subtract, op1=ALU.mult),
                 reads=[b_mv[sbi]], writes=b_v[sbi])
            R.op("dve", lambda h, sbi=sbi: h.tensor_tensor(out=vn[sbi][:], in0=v[sbi][:], in1=Gbc[:], op=ALU.mult),
                 reads=b_v[sbi] + [b_Gbc], writes=[b_vn[sbi]])

    def p2_SGmm(t):
        for sbi in range(NSB):
            bg, bbg = next_big()
            R.group("pe", [(lambda h, hd=hd, sbi=sbi, bg=bg: h.matmul(bg[:, hd, :], lhsT=vn[sbi][:, hd * 128:(hd + 1) * 128],
                                                                      rhs=WmT[:, hd, :], start=True, stop=True))
                           for hd in range(8)], reads=[b_vn[sbi], b_WmT], writes=[bbg])
            R.op("dve", lambda h, sbi=sbi, bg=bg: h.tensor_tensor(out=tmpS[sbi][:], in0=bg[:], in1=Cst[:], op=ALU.add),
                 reads=[bbg, b_Cst], writes=[b_tmpS[sbi]])
            R.op("dve", lambda h, sbi=sbi: h.tensor_tensor(out=sT[:, :, sbi * 128:(sbi + 1) * 128], in0=tmpS[sbi][:],
                                                            in1=uT[:, :, sbi * 128:(sbi + 1) * 128], op=ALU.mult),
                 reads=[b_tmpS[sbi]] + b_uT, writes=[b_sT[sbi]])

    def p2_SGO(t):
        ts = t % 2
        for jo in range(KC):
            pb, bpb = next_bank2()
            terms = [(Bbc2[:, BI["b_sgo"], jo * 128:(jo + 1) * 128], ones_t[:, 0:TT], [b_Bbc, b_ones])]
            terms += [(Wsgo[:, k, jo * 128:(jo + 1) * 128], sT[:, k, :], [b_Wsgo[k]] + b_sT) for k in range(KC)]
            mm_group(pb[:, 0:TT], bpb, terms)
            s2 = jo % 2
            R.op("dve", lambda h, jo=jo, pb=pb, s2=s2: h.scalar_tensor_tensor(out=t1[s2][:], in0=tgb[:, jo, :], scalar=1.0,
                                                                              in1=pb[:, 0:TT], op0=ALU.add, op1=ALU.mult),
                 reads=[b_tgb[jo], bpb], writes=[b_t1[s2]])
            R.op("dve", lambda h, jo=jo, s2=s2: h.tensor_tensor(out=mixT[:, jo, :], in0=t1[s2][:], in1=gaya[ts][:, jo, :],
                                                                 op=ALU.add),
                 reads=[b_t1[s2], b_gaya[ts]], writes=[b_mixT[jo]])

    def p2_OUT(t):
        for sbi in range(NSB):
            xsl = (t % 3) * NSB + sbi
            for n in range(2):
                px, bpx = next_bank2()
                terms = [(mixT[:, k, sbi * 128:(sbi + 1) * 128], Wout[:, k, n * 512:(n + 1) * 512], [b_mixT[k], b_Wout[k]])
                         for k in range(KC)]
                mm_group(px[:, 0:512], bpx, terms)
                R.op("dve", lambda h, xsl=xsl, n=n, px=px: h.tensor_tensor(out=xs2[xsl][:, n * 512:(n + 1) * 512], in0=px[:, 0:512],
                                                                           in1=xs2[xsl][:, n * 512:(n + 1) * 512], op=ALU.add),
                     reads=[bpx], writes=[b_xs2[xsl]])

    def p2_route(gsb, s, pl):
        r = rt[s]
        B = lambda n: r["b_" + n]
        R.op("dve", lambda h: h.tensor_copy(out=r["L"][:], in_=pl[0][:, 0:36]), reads=[pl[1]], writes=[B("L")])
        R.op("dve", lambda h: h.tensor_copy(out=r["gl8"][:, 0:4], in_=r["L"][:, 0:4]), reads=[B("L")], writes=[B("gl8")])
        R.op("dve", lambda h: h.max(out=r["t8g"][:], in_=r["gl8"][:]), reads=[B("gl8")], writes=[B("t8g")])
        R.op("dve", lambda h: h.max_index(out=r["i8g"][:], in_max=r["t8g"][:], in_values=r["gl8"][:]),
             reads=[B("gl8"), B("t8g")], writes=[B("i8g")])
        R.op("dve", lambda h: h.tensor_scalar(out=GLM[:, gsb, :], in0=r["L"][:, 0:4], scalar1=r["t8g"][:, 0:1], scalar2=None,
                                              op0=ALU.subtract), reads=[B("L"), B("t8g")], pwrites=[b_GLM])
        R.op("dve", lambda h: h.tensor_copy(out=r["gself"][:], in_=r["i8g"][:, 0:1]), reads=[B("i8g")], writes=[B("gself")])
        R.op("dve", lambda h: h.tensor_scalar(out=r["pen32"][:], in0=iotg32[:], scalar1=r["gself"][:, 0:1], scalar2=None,
                                              op0=ALU.is_equal), reads=[B("gself"), b_rc], writes=[B("pen32")])
        R.op("dve", lambda h: h.tensor_scalar(out=r["pen32"][:], in0=r["pen32"][:], scalar1=1.0, scalar2=1e30,
                                              op0=ALU.subtract, op1=ALU.mult), reads=[B("pen32")], writes=[B("pen32")])
        R.op("dve", lambda h: h.tensor_tensor(out=r["M32"][:], in0=r["L"][:, 4:36], in1=r["pen32"][:], op=ALU.add),
             reads=[B("L"), B("pen32")], writes=[B("M32")])
        R.op("dve", lambda h: h.max(out=r["t8e"][:], in_=r["M32"][:]), reads=[B("M32")], writes=[B("t8e")])
        R.op("dve", lambda h: h.max_index(out=r["i8e"][:], in_max=r["t8e"][:], in_values=r["M32"][:]),
             reads=[B("M32"), B("t8e")], writes=[B("i8e")])
        R.op("dve", lambda h: h.tensor_tensor(out=DV[:, gsb:gsb + 1], in0=r["t8e"][:, 0:1], in1=r["t8e"][:, 1:2], op=ALU.subtract),
             reads=[B("t8e")], pwrites=[b_DV])
        R.op("dve", lambda h: h.tensor_copy(out=r["ef"][:], in_=r["i8e"][:, 0:2]), reads=[B("i8e")], writes=[B("ef")])
        R.op("dve", lambda h: h.tensor_scalar(out=r["A0"][:], in0=iotx32[:], scalar1=r["ef"][:, 0:1], scalar2=None,
                                              op0=ALU.is_equal), reads=[B("ef"), b_rc], writes=[B("A0")])
        R.op("dve", lambda h: h.tensor_scalar(out=r["A1"][:], in0=iotx32[:], scalar1=r["ef"][:, 1:2], scalar2=None,
                                              op0=ALU.is_equal), reads=[B("ef"), b_rc], writes=[B("A1")])
        R.op("dve", lambda h: h.tensor_tensor(out=r["Ab"][:], in0=r["A0"][:], in1=r["A1"][:], op=ALU.add),
             reads=[B("A0"), B("A1")], writes=[B("Ab")])

    def p2_route_b(gsb, s):
        r = rt[s]
        B = lambda n: r["b_" + n]
        ps, bps = next_bank2()
        tt_ = gsb // NSB
        sbi_ = gsb % NSB
        a_in, a_out = tt_ % 2, (tt_ + 1) % 2
        terms = [(Ustrict[:], r["Ab"][:], [b_rc, B("Ab")]), (ones_t[:, 0:128], Acum[a_in][:], [b_ones, b_Acum[a_in]])]
        for pj in range(sbi_):
            rp = rt[(gsb - sbi_ + pj) % 4]
            terms.append((ones_t[:, 0:128], rp["Ab"][:], [b_ones, rp["b_Ab"]]))
        mm_group(ps[:, 0:32], bps, terms)
        if sbi_ == NSB - 1:
            for pj in range(NSB):
                rp = rt[(gsb - sbi_ + pj) % 4]
                src = Acum[a_in] if pj == 0 else Acum[a_out]
                bsrc = b_Acum[a_in] if pj == 0 else b_Acum[a_out]
                R.op("dve", lambda h, rp=rp, src=src: h.tensor_tensor(out=Acum[a_out][:], in0=src[:], in1=rp["Ab"][:], op=ALU.add),
                     reads=[bsrc, rp["b_Ab"]], writes=[b_Acum[a_out]])
        for kk, An in enumerate(["A0", "A1"]):
            R.op("dve", lambda h, An=An: h.tensor_tensor(out=r["prod"][:], in0=r[An][:], in1=ps[:, 0:32], op=ALU.mult),
                 reads=[B(An), bps], writes=[B("prod")])
            R.op("dve", lambda h, kk=kk: h.tensor_reduce(out=r["pos"][:, kk:kk + 1], in_=r["prod"][:], axis=AX.X, op=ALU.add),
                 reads=[B("prod")], pwrites=[B("pos")])
        R.op("dve", lambda h: h.tensor_scalar(out=r["pos"][:], in0=r["pos"][:], scalar1=float(CAP - 1), scalar2=None, op0=ALU.min),
             reads=[B("pos")], writes=[B("pos")])
        R.op("dve", lambda h: h.scalar_tensor_tensor(out=r["dest"][:], in0=r["ef"][:], scalar=float(CAP), in1=r["pos"][:],
                                                     op0=ALU.mult, op1=ALU.add), reads=[B("ef"), B("pos")], writes=[B("dest")])
        R.op("dve", lambda h: h.tensor_copy(out=DEST[:, gsb, :], in_=r["dest"][:]), reads=[B("dest")], writes=[b_DEST[gsb]])
        for kk in range(2):
            R.dma("pool", "sc%d_%d" % (s, kk), lambda h, kk=kk: h.indirect_dma_start(
                out=xpad[:, :], out_offset=bass.IndirectOffsetOnAxis(ap=DEST[:, gsb, kk:kk + 1], axis=0),
                in_=h2b[s][:], in_offset=None), reads=[b_h2b[s], b_DEST[gsb]], pwrites=[b_xpad])

    def p2_tail(t):
        for sbi in range(NSB):
            xsl = (t % 3) * NSB + sbi
            gsb = t * NSB + sbi
            s = gsb % 2
            R.dma("sp", "x1s%d" % xsl, lambda h, xsl=xsl, gsb=gsb: h.dma_start(out=x1s[gsb * 128:(gsb + 1) * 128, :], in_=xs2[xsl][:]),
                  reads=[b_xs2[xsl]], writes=[b_x1s[gsb]])
            rms_head(xs2[xsl][:], b_xs2[xsl], st2[xsl][:, 2:3], st2[xsl][:, 3:4], b_st2[xsl])
            R.op("dve", lambda h, xsl=xsl, s=s: h.tensor_scalar(out=h2f[s][:], in0=xs2[xsl][:], scalar1=st2[xsl][:, 3:4],
                                                                 scalar2=None, op0=ALU.mult),
                 reads=[b_xs2[xsl], b_st2[xsl]], writes=[b_h2f[s]])
            R.op("act", lambda h, xsl=xsl, gsb=gsb: h.activation(out=h2b[gsb % 4][:], in_=xs2[xsl][:], func=AF.Identity,
                                                                 scale=st2[xsl][:, 3:4]),
                 reads=[b_xs2[xsl], b_st2[xsl]], writes=[b_h2b[gsb % 4]])

    def p2_tail_b(t):
        pls = []
        for sbi in range(NSB):
            gsb = t * NSB + sbi
            s = gsb % 2
            bg, bbg = next_big()
            R.group("pe", [(lambda h, k=k, s=s, bg=bg: h.transpose(out=bg[:, k, :], in_=h2f[s][:, k * 128:(k + 1) * 128],
                                                                   identity=identf[:])) for k in range(KC)],
                    reads=[b_h2f[s], b_identf], writes=[bbg])
            R.op("act", lambda h, s=s, bg=bg: h.activation(out=h2T[s][:], in_=bg[:], func=AF.Copy), reads=[bbg], writes=[b_h2T[s]])
            pl = next_bank2()
            terms = [(onesf[:], rbbc[:, 0:36], [b_ones, b_rows])]
            terms += [(h2T[s][:, k, :], Wr[:, k, :], [b_h2T[s], b_Wr]) for k in range(KC)]
            mm_group(pl[0][:, 0:36], pl[1], terms)
            pls.append(pl)
        for sbi in range(NSB):
            gsb = t * NSB + sbi
            p2_route(gsb, gsb % 4, pls[sbi])

    p2_gaya(0)
    p2_head_a(0)
    p2_head_b(0)
    if NT > 1:
        p2_head_a(1)
    p2_V(0)
    p2_LN(0)
    p2_U(0)
    p2_GB(0)
    for t in range(NT):
        nxt = t + 1 < NT
        p2_SGmm(t)
        if nxt:
            p2_head_b(t + 1)
            p2_V(t + 1)
        p2_SGO(t)
        if nxt:
            p2_LN(t + 1)
            p2_gaya(t + 1)
            p2_U(t + 1)
        p2_OUT(t)
        p2_tail(t)
        if t + 2 < NT:
            p2_head_a(t + 2)
        if nxt:
            p2_GB(t + 1)
        p2_tail_b(t)
        if t > 0:
            for sbi in range(NSB):
                p2_route_b((t - 1) * NSB + sbi, ((t - 1) * NSB + sbi) % 4)
    for sbi in range(NSB):
        p2_route_b((NT - 1) * NSB + sbi, ((NT - 1) * NSB + sbi) % 4)
    R.op("act", lambda h: h.activation(out=GLM[:], in_=GLM[:], func=AF.Exp), reads=[b_GLM], writes=[b_GLM])
    R.op("dve", lambda h: h.tensor_reduce(out=EW1[:], in_=GLM[:], axis=AX.X, op=ALU.add), reads=[b_GLM], writes=[b_EW])
    R.op("dve", lambda h: h.reciprocal(out=EW1[:], in_=EW1[:]), reads=[b_EW], writes=[b_EW])
    R.op("act", lambda h: h.activation(out=EW0[:], in_=DV[:], func=AF.Sigmoid), reads=[b_DV, b_EW], writes=[b_EW])
    R.op("dve", lambda h: h.tensor_tensor(out=EW0[:], in0=EW0[:], in1=EW1[:], op=ALU.mult), reads=[b_EW], writes=[b_EW])
    R.op("dve", lambda h: h.tensor_tensor(out=EW1[:], in0=EW1[:], in1=EW0[:], op=ALU.subtract), reads=[b_EW], writes=[b_EW])
    if debug:
        R.dma("sp", "dbg0", lambda h: h.dma_start(out=dbg_dest[:, :], in_=DEST[:].rearrange("p a b -> p (a b)")), reads=b_DEST)
        R.dma("sp", "dbg1", lambda h: h.dma_start(out=dbg_ew[:, 0:NGSB], in_=EW0[:]), reads=[b_EW])
        R.dma("sp", "dbg2", lambda h: h.dma_start(out=dbg_ew[:, NGSB:2 * NGSB], in_=EW1[:]), reads=[b_EW])
    barrier()
    A2.free()

    A3 = Alloc(nc)
    g2c = cols
    stg_g = A3.sb("stg_g", [128, KC, 512], F32)
    stg_u = A3.sb("stg_u", [128, KC, 512], F32)
    stg_d = A3.sb("stg_d", [128, 4, D], F32)
    Wg_b = [A3.sb("Wg_b%d" % i, [128, KC, 512], BF16) for i in range(2)]
    Wu_b = [A3.sb("Wu_b%d" % i, [128, KC, 512], BF16) for i in range(2)]
    Wd_b = [A3.sb("Wd_b%d" % i, [128, 4, D], BF16) for i in range(2)]
    xp = [A3.sb("xp%d" % i, [128, NB, D], BF16) for i in range(2)]
    xT = A3.sb("xT", [128, KC, CAP], BF16)
    sgt = [A3.sb("sgt%d" % i, [128, 512], F32) for i in range(2)]
    hTe = A3.sb("hTe", [128, 4, CAP], BF16)
    NYB = 8
    ybuf = [A3.sb("ybuf%d" % i, [128, D], F32) for i in range(NYB)]
    trE = [A3.ps("trE%d" % i, [128, KC, 128], BF16) for i in range(2)]
    mmE = [A3.ps("mmE%d" % i, [128, 512], F32) for i in range(6)]
    b_stg = {n: Buf("stg_" + n) for n in "gud"}
    b_We = [{n: Buf("W%s%d" % (n, i)) for n in "gud"} for i in range(2)]
    b_xp = [Buf("xp0"), Buf("xp1")]
    b_xT = [Buf("xT%d" % b) for b in range(NB)]
    b_sgt = [Buf("sgt0"), Buf("sgt1")]
    b_hTe = [Buf("hTe%d" % i) for i in range(8)]
    b_ybuf = [Buf("ybuf%d" % i) for i in range(NYB)]
    b_trE = [Buf("trE0"), Buf("trE1")]
    b_mmE = [Buf("mmE%d" % i) for i in range(6)]
    b_ypad = [Buf("ypad%d" % e) for e in range(NE)]
    _me = [0]

    def next_bankE():
        _me[0] = (_me[0] + 1) % len(mmE)
        return mmE[_me[0]], b_mmE[_me[0]]

    halves = []
    r0 = 0
    while r0 < CAP:
        r1 = min(CAP, r0 + 512)
        halves.append((r0, r1))
        r0 = r1
    _yb = [0]

    def dma_g(e):
        R.dma("sp", "stg_g", lambda h: h.dma_start(out=stg_g[:], in_=w_gate[e].rearrange("(k p) f -> p k f", p=128)),
              writes=[b_stg["g"]])

    def dma_u(e):
        R.dma("sp", "stg_u", lambda h: h.dma_start(out=stg_u[:], in_=w_up[e].rearrange("(k p) f -> p k f", p=128)),
              writes=[b_stg["u"]])

    def dma_d(e):
        R.dma("sp", "stg_d", lambda h: h.dma_start(out=stg_d[:], in_=w_down[e].rearrange("(k p) f -> p k f", p=128)),
              writes=[b_stg["d"]])

    def dma_xp(e):
        sl = e % 2
        R.dma("sp", "xp%d" % sl, lambda h: h.dma_start(
            out=xp[sl][:], in_=xpad[e * CAP:(e + 1) * CAP, :].rearrange("(b p) d -> p b d", p=128)),
            reads=[b_xpad], writes=[b_xp[sl]])

    def cast_g(e, k):
        sl = e % 2
        cast_op("act", Wg_b[sl][:, k, :], stg_g[:, k, :], col("g2", k), [b_stg["g"], b_cols], [b_We[sl]["g"]])

    def cast_u(e, k):
        sl = e % 2
        cast_op("act" if k % 2 else "dve", Wu_b[sl][:, k, :], stg_u[:, k, :], col("g2", k), [b_stg["u"], b_cols], [b_We[sl]["u"]])

    def cast_d(e, m):
        sl = e % 2
        cast_op("dve", Wd_b[sl][:, m, :], stg_d[:, m, :], None, [b_stg["d"]], [b_We[sl]["d"]])

    def moe_thunks(e):
        L = []
        if e + 2 < NE:
            L.append(lambda: dma_xp(e + 2))
        if e + 1 < NE:
            L += [(lambda k=k: cast_g(e + 1, k)) for k in range(KC)]
        if e + 2 < NE:
            L.append(lambda: dma_g(e + 2))
        if e + 1 < NE:
            L += [(lambda k=k: cast_u(e + 1, k)) for k in range(KC)]
        if e + 2 < NE:
            L.append(lambda: dma_u(e + 2))
        if e + 1 < NE:
            L += [(lambda m=m: cast_d(e + 1, m)) for m in range(4)]
        if e + 2 < NE:
            L.append(lambda: dma_d(e + 2))
        return L

    def moe_compute_a(e, L):
        sl = e % 2
        for blk in range(NB):
            ti = blk % 2
            R.group("pe", [(lambda h, k=k, blk=blk, ti=ti: h.transpose(out=trE[ti][:, k, :], in_=xp[sl][:, blk, k * 128:(k + 1) * 128],
                                                                      identity=identb[:])) for k in range(KC)],
                    reads=[b_xp[sl], b_identb], writes=[b_trE[ti]])
            if blk % 2 == 0:
                R.op("act", lambda h, blk=blk, ti=ti: h.activation(out=xT[:, :, blk * 128:(blk + 1) * 128], in_=trE[ti][:], func=AF.Copy),
                     reads=[b_trE[ti]], writes=[b_xT[blk]])
            else:
                R.op("dve", lambda h, blk=blk, ti=ti: h.tensor_copy(out=xT[:, :, blk * 128:(blk + 1) * 128], in_=trE[ti][:]),
                     reads=[b_trE[ti]], writes=[b_xT[blk]])
        for hi, (r0, r1) in enumerate(halves):
            n = r1 - r0
            xbufs = [b_xT[b] for b in range(r0 // 128, (r1 + 127) // 128)]
            for m in range(4):
                pg, bpg = next_bankE()
                mm_group(pg[:, 0:n], bpg, [(Wg_b[sl][:, k, m * 128:(m + 1) * 128], xT[:, k, r0:r1], [b_We[sl]["g"]] + xbufs)
                                           for k in range(KC)])
                pu, bpu = next_bankE()
                mm_group(pu[:, 0:n], bpu, [(Wu_b[sl][:, k, m * 128:(m + 1) * 128], xT[:, k, r0:r1], [b_We[sl]["u"]] + xbufs)
                                           for k in range(KC)])
                s2 = m % 2
                R.op("act", lambda h, pg=pg, s2=s2, n=n: h.activation(out=sgt[s2][:, 0:n], in_=pg[:, 0:n], func=AF.Silu),
                     reads=[bpg], writes=[b_sgt[s2]])
                R.op("dve", lambda h, pu=pu, s2=s2, n=n, m=m, r0=r0, r1=r1: h.tensor_tensor(
                    out=hTe[:, m, r0:r1], in0=sgt[s2][:, 0:n], in1=pu[:, 0:n], op=ALU.mult),
                    reads=[b_sgt[s2], bpu], writes=[b_hTe[hi * 4 + m]])
                if L:
                    L.pop(0)()

    def moe_compute_b(e, L):
        sl = e % 2
        for blk in range(NB):
            hi = (blk * 128) // 512
            _yb[0] = (_yb[0] + 1) % NYB
            ys = _yb[0]
            for n2 in range(2):
                py, bpy = next_bankE()
                mm_group(py[:, 0:512], bpy, [(hTe[:, m, blk * 128:(blk + 1) * 128], Wd_b[sl][:, m, n2 * 512:(n2 + 1) * 512],
                                              [b_hTe[hi * 4 + m], b_We[sl]["d"]]) for m in range(4)])
                if n2 == 0:
                    R.op("act", lambda h, py=py, ys=ys: h.activation(out=ybuf[ys][:, 0:512], in_=py[:, 0:512], func=AF.Copy),
                         reads=[bpy], writes=[b_ybuf[ys]])
                else:
                    R.op("dve", lambda h, py=py, ys=ys: h.tensor_copy(out=ybuf[ys][:, 512:1024], in_=py[:, 0:512]),
                         reads=[bpy], pwrites=[b_ybuf[ys]])
                if L:
                    L.pop(0)()
            R.dma("pool", "yst%d" % ys, lambda h, blk=blk, ys=ys: h.dma_start(
                out=ypad[e * CAP + blk * 128:e * CAP + (blk + 1) * 128, :], in_=ybuf[ys][:]),
                reads=[b_ybuf[ys]], pwrites=[b_ypad[e]])

    dma_xp(0); dma_g(0); dma_u(0); dma_d(0)
    for k in range(KC):
        cast_g(0, k)
    dma_g(1)
    for k in range(KC):
        cast_u(0, k)
    dma_u(1)
    for m in range(4):
        cast_d(0, m)
    dma_d(1)
    dma_xp(1)
    for e in range(NE):
        L = moe_thunks(e)
        moe_compute_a(e, L)
        moe_compute_b(e, L)
        while L:
            L.pop(0)()
    barrier()
    A3.free()

    A4 = Alloc(nc)
    Gfbc = A4.sb("Gfbc", [128, D], F32)
    GRP = 4
    NCB = 2 * GRP
    x1t = [A4.sb("x1t%d" % i, [128, D], F32) for i in range(NCB)]
    y0 = [A4.sb("y0_%d" % i, [128, D], F32) for i in range(NCB)]
    y1 = [A4.sb("y1_%d" % i, [128, D], F32) for i in range(NCB)]
    st3 = [A4.sb("st3_%d" % i, [128, 2, GRP], F32) for i in range(2)]
    b_Gf = Buf("Gf")
    b_x1t = [Buf("x1t%d" % i) for i in range(NCB)]
    b_y0 = [Buf("y0_%d" % i) for i in range(NCB)]
    b_y1 = [Buf("y1_%d" % i) for i in range(NCB)]
    b_st3 = [Buf("st3_%d" % i) for i in range(2)]
    R.dma("sp", "c9", lambda h: h.dma_start(out=Gfbc[:], in_=bvecs_d[BV["g_f"]:BV["g_f"] + 1, :].partition_broadcast(128)), writes=[b_Gf])

    def cmb_load(gsb):
        s = gsb % NCB
        R.dma("sp", "x1t%d" % s, lambda h: h.dma_start(out=x1t[s][:], in_=x1s[gsb * 128:(gsb + 1) * 128, :]),
              reads=[b_x1s[gsb]], writes=[b_x1t[s]])
        R.dma("pool", "g0_%d" % s, lambda h: h.indirect_dma_start(
            out=y0[s][:], out_offset=None, in_=ypad[:, :],
            in_offset=bass.IndirectOffsetOnAxis(ap=DEST[:, gsb, 0:1], axis=0)), reads=b_ypad + [b_DEST[gsb]], writes=[b_y0[s]])
        R.dma("pool", "g1_%d" % s, lambda h: h.indirect_dma_start(
            out=y1[s][:], out_offset=None, in_=ypad[:, :],
            in_offset=bass.IndirectOffsetOnAxis(ap=DEST[:, gsb, 1:2], axis=0)), reads=b_ypad + [b_DEST[gsb]], writes=[b_y1[s]])

    def cmb_group(g):
        gs = list(range(g * GRP, min(NGSB, (g + 1) * GRP)))
        sg = g % 2
        for i, gsb in enumerate(gs):
            s = gsb % NCB
            R.op("dve", lambda h, s=s, gsb=gsb: h.scalar_tensor_tensor(out=x1t[s][:], in0=y0[s][:], scalar=EW0[:, gsb:gsb + 1],
                                                                       in1=x1t[s][:], op0=ALU.mult, op1=ALU.add),
                 reads=[b_y0[s], b_EW], writes=[b_x1t[s]])
            R.op("dve", lambda h, s=s, gsb=gsb: h.scalar_tensor_tensor(out=x1t[s][:], in0=y1[s][:], scalar=EW1[:, gsb:gsb + 1],
                                                                       in1=x1t[s][:], op0=ALU.mult, op1=ALU.add),
                 reads=[b_y1[s], b_EW], writes=[b_x1t[s]])
            _jk[0] ^= 1
            jk = _jk[0]
            R.op("act", lambda h, s=s, i=i, jk=jk: h.activation(out=junk[jk][:], in_=x1t[s][:], func=AF.Square,
                                                                accum_out=st3[sg][:, 0, i:i + 1]),
                 reads=[b_x1t[s]], writes=[b_junk[jk]], pwrites=[b_st3[sg]])
        R.op("dve", lambda h: h.tensor_scalar(out=st3[sg][:, 0, :], in0=st3[sg][:, 0, :], scalar1=1.0 / D, scalar2=EPS,
                                              op0=ALU.mult, op1=ALU.add), reads=[b_st3[sg]], writes=[b_st3[sg]])
        R.op("pool", lambda h: h.tensor_tensor(out=st3[sg][:, 1, :], in0=st3[sg][:, 0, :], in1=negh[:, 0:GRP], op=ALU.pow),
             reads=[b_st3[sg], b_negh], writes=[b_st3[sg]])
        for i, gsb in enumerate(gs):
            s = gsb % NCB
            R.op("dve", lambda h, s=s, i=i: h.scalar_tensor_tensor(out=x1t[s][:], in0=x1t[s][:], scalar=st3[sg][:, 1, i:i + 1],
                                                                   in1=Gfbc[:], op0=ALU.mult, op1=ALU.mult),
                 reads=[b_st3[sg], b_Gf], writes=[b_x1t[s]])
            R.dma("sp", "ot%d" % s, lambda h, s=s, gsb=gsb: h.dma_start(out=out[gsb * 128:(gsb + 1) * 128, :], in_=x1t[s][:]),
                  reads=[b_x1t[s]])

    NG = (NGSB + GRP - 1) // GRP
    for gsb in range(min(GRP, NGSB)):
        cmb_load(gsb)
    for g in range(NG):
        for gsb in range((g + 1) * GRP, min(NGSB, (g + 2) * GRP)):
            cmb_load(gsb)
        cmb_group(g)
    barrier()
    R.emit()
    A4.free()
    PA.free()
    R.close()
    return nc


_NC_CACHE = {}


def _layout_inputs(inp):
    f = lambda a: np.ascontiguousarray(np.asarray(a, dtype=np.float32))
    vec8 = lambda v: f(np.asarray(v).reshape(8, 128).T)
    b_in = np.asarray(inp["b_in"])[0]
    colmap = {
        "g1": inp["norm_mix_g"][0], "b_a2": b_in[1024:2048], "conv_b": inp["conv_b"][0], "ln_g": inp["conv_ln_g"][0],
        "ln_b": inp["conv_ln_b"][0], "b_u": b_in[2048:3072], "b_ga": b_in[4096:5120], "b_gb": b_in[5120:6144],
        "sg_b": inp["sg_ln_b"][0], "g2": inp["norm_ffn_g"][0],
    }
    cols = f(np.concatenate([vec8(colmap[n]) for n in COLV], axis=1))
    convwT = f(np.asarray(inp["conv_w"])[0].reshape(31, 8, 128).transpose(2, 1, 0).reshape(128, 8 * 31))
    wsgT = f(np.asarray(inp["w_sg"])[0].transpose(2, 0, 1).reshape(128, 8 * 128))
    blk = np.arange(128) // 64
    maskT = f((blk[None, :] >= blk[:, None]).astype(np.float32))
    rb = f(np.concatenate([np.asarray(inp["b_router_group"])[0], np.asarray(inp["b_router_expert"])[0]])[None, :])
    bvecs = f(np.stack([np.asarray(inp["sg_ln_g"])[0], np.asarray(inp["b_out"])[0], np.asarray(inp["norm_final_g"]),
                        np.asarray(inp["b_sg"])[0].reshape(-1), b_in[0:1024], np.asarray(inp["b_conv_out"])[0],
                        np.asarray(inp["b_sg_out"])[0], b_in[3072:4096]]))
    wr = f(np.concatenate([np.asarray(inp["w_router_group"])[0], np.asarray(inp["w_router_expert"])[0]], axis=1))
    shared = {
        "w_in": f(inp["w_in"][0]), "w_co": f(inp["w_conv_out"][0]), "w_sgo": f(inp["w_sg_out"][0]), "w_out": f(inp["w_out"][0]),
        "w_gate": f(inp["w_expert_gate"][0]), "w_up": f(inp["w_expert_up"][0]), "w_down": f(inp["w_expert_down"][0]),
        "cols": cols, "convwT": convwT, "wsgT": wsgT, "maskT": maskT, "rb": rb, "bvecs": bvecs, "wr": wr,
    }
    return shared


def kernel(**inputs):
    x = np.asarray(inputs["x"], dtype=np.float32)
    B, S, Dm = x.shape
    ncores = 8
    nseq = B // ncores
    key = (nseq, S)
    if key not in _NC_CACHE:
        _NC_CACHE[key] = build_nc(NSEQ=nseq, S=S, NB=8)
    nc = _NC_CACHE[key]
    shared = _layout_inputs(inputs)
    in_maps = []
    for c in range(ncores):
        m = dict(shared)
        m["x"] = np.ascontiguousarray(x[c * nseq:(c + 1) * nseq].reshape(nseq * S, Dm))
        in_maps.append(m)
    res = run_bass_kernel_spmd(nc, in_maps, core_ids=list(range(ncores)))
    outs = [np.asarray(r["out"]).reshape(nseq, S, Dm) for r in res.results]
    return np.concatenate(outs, axis=0).astype(np.float32)
```
